# Optimizing a Trainium2 kernel written in Bass

```python
import math
import jax
import jax.numpy as jnp
from jax import lax
import numpy as np

D_MODEL = 1024
BATCH = 8
SEQ = 4096
DEPTH = 1

CTX_LEN = 256
GRID_W = 64
N_MOD = 6
EPS = 1e-6
DA_HEADS = 8
DA_QK = 64
DA_V = 2 * DA_QK
DA_QK_COLS = DA_HEADS * 2 * DA_QK
DA_WIDTH = DA_HEADS * DA_V
ROPE_THETA = 10000.0
ROPE_FREQS = DA_QK // 4
Q_BLOCK = 128
RW_HEADS = 16
RW_HEAD = 64
RW_WIDTH = RW_HEADS * RW_HEAD
RW_W_LORA = 64
RW_A_LORA = 64
RW_G_LORA = 128
RW_SHIFT_CH = 3 * RW_WIDTH + RW_W_LORA + RW_A_LORA + RW_G_LORA
RW_LN_EPS = 64e-5
L2_EPS = 1e-12
RW_SPLIT_IDX = (RW_WIDTH, 2 * RW_WIDTH, 3 * RW_WIDTH, 3 * RW_WIDTH + RW_W_LORA, 3 * RW_WIDTH + RW_W_LORA + RW_A_LORA)
N_BRANCH = 2
IN_SPLIT_IDX = (DA_QK_COLS, 2 * DA_QK_COLS, 2 * DA_QK_COLS + DA_WIDTH, 2 * DA_QK_COLS + DA_WIDTH + RW_SHIFT_CH)
IN_WIDTH = 2 * DA_QK_COLS + DA_WIDTH + RW_SHIFT_CH + N_BRANCH * D_MODEL
PEER_HEADS = 8
N_KEYS = 128
N_EXPERTS = N_KEYS * N_KEYS
PEER_TOPK = 16
PEER_QDIM = 256
PEER_CHUNK = 128

kernel_name = 'diffattn_rwkv7_peer_hybrid_dit'


def rmsnorm(x, g):
    xf = x.astype(jnp.float32)
    y = xf * lax.rsqrt(jnp.mean(xf * xf, axis=-1, keepdims=True) + EPS)
    return (y * g.astype(jnp.float32)).astype(x.dtype)


def modulate(h, shift, scale):
    return h * (1 + scale) + shift


def axial_angles(n_tokens):
    rows = n_tokens // GRID_W
    row = jnp.repeat(jnp.arange(rows, dtype=jnp.float32), GRID_W)
    col = jnp.tile(jnp.arange(GRID_W, dtype=jnp.float32), rows)
    inv = ROPE_THETA ** (-jnp.arange(ROPE_FREQS, dtype=jnp.float32) / ROPE_FREQS)
    return jnp.stack([row[:, None] * inv, col[:, None] * inv], axis=1)


def apply_axial_rope(x, ang):
    xs = x.reshape(x.shape[:-1] + (2, 2, ROPE_FREQS))
    cos = jnp.cos(ang)[:, None, None, :, None, :].astype(x.dtype)
    sin = jnp.sin(ang)[:, None, None, :, None, :].astype(x.dtype)
    x1, x2 = xs[..., 0:1, :], xs[..., 1:2, :]
    out = jnp.concatenate([x1 * cos - x2 * sin, x2 * cos + x1 * sin], axis=-2)
    return out.reshape(x.shape)


def diff_heads(q, k, v, q_g, k_g, ang):
    B, T, _ = q.shape
    q = rmsnorm(q.reshape(B, T, DA_HEADS, 2, DA_QK), q_g)
    k = rmsnorm(k.reshape(B, T, DA_HEADS, 2, DA_QK), k_g)
    if ang is not None:
        q = apply_axial_rope(q, ang)
        k = apply_axial_rope(k, ang)
    q = q.transpose(0, 3, 2, 1, 4)
    k = k.transpose(0, 3, 2, 1, 4)
    v = v.reshape(B, T, DA_HEADS, DA_V).transpose(0, 2, 1, 3)
    return q, k, v


def diff_attention(q, k, v, lam):
    B, _, H, T, d = q.shape
    nb = T // Q_BLOCK
    qb = q.reshape(B, 2, H, nb, Q_BLOCK, d).transpose(3, 0, 1, 2, 4, 5)
    scale = d ** -0.5

    def one_block(q_blk):
        s = jnp.einsum('bmhqd,bmhkd->bmhqk', q_blk, k).astype(jnp.float32) * scale
        p = jax.nn.softmax(s, axis=-1)
        pd = (p[:, 0] - lam * p[:, 1]).astype(v.dtype)
        return jnp.einsum('bhqk,bhkd->bhqd', pd, v)

    o = lax.map(one_block, qb)
    return o.transpose(1, 0, 3, 2, 4).reshape(B, T, H * v.shape[-1])


def diff_post(o, out_g, lam_init):
    B, T, _ = o.shape
    o = rmsnorm(o.reshape(B, T, DA_HEADS, DA_V), out_g) * (1 - lam_init)
    return o.reshape(B, T, DA_WIDTH)


def centred_shift(z, w):
    zp = jnp.pad(z, ((0, 0), (1, 1), (0, 0)))
    return w[0] * zp[:, :-2] + w[1] * zp[:, 1:-1] + w[2] * zp[:, 2:]


def head_l2norm(t):
    th = t.reshape(t.shape[:-1] + (RW_HEADS, RW_HEAD)).astype(jnp.float32)
    th = th * lax.rsqrt(jnp.sum(th * th, axis=-1, keepdims=True) + L2_EPS)
    return th.reshape(t.shape).astype(t.dtype)


def dual(t):
    return jnp.stack([t, t])


def to_scan(t):
    t = jnp.stack([t[0], t[1][:, ::-1]]).astype(jnp.float32)
    return t.reshape(t.shape[:3] + (RW_HEADS, RW_HEAD)).transpose(2, 0, 1, 3, 4)


def from_scan(y):
    y = y.transpose(1, 2, 0, 3, 4)
    return y[0] + y[1][:, ::-1]


def rwkv_prepare(z, shift_w, w0, w_up, a0, a_up, k_k, k_a):
    z = centred_shift(z, shift_w)
    r, k, v, wd, ad, gd = jnp.split(z, RW_SPLIT_IDX, axis=-1)
    w_log = -jax.nn.softplus(-(w0[:, None, None, :] + jnp.einsum('btr,drc->dbtc', jnp.tanh(wd), w_up))) - 0.5
    decay = jnp.exp(-jnp.exp(w_log.astype(jnp.float32)))
    a = jax.nn.sigmoid(a0[:, None, None, :] + jnp.einsum('btr,drc->dbtc', ad, a_up))
    kk = head_l2norm(k * k_k)
    k_eff = k[None] * (1 + (a - 1) * k_a)
    scan_in = (to_scan(decay), to_scan(dual(kk)), to_scan(a), to_scan(k_eff), to_scan(dual(v)), to_scan(dual(r)))
    return scan_in, (r, k, v, gd)


def wkv_scan(s0, scan_in):
    def step(s, inp):
        w, kk, a, k, v, r = inp
        sa = jnp.einsum('dbhij,dbhj->dbhi', s, kk)
        s = s * w[..., None, :] - sa[..., :, None] * (kk * a)[..., None, :] + v[..., :, None] * k[..., None, :]
        return s, jnp.einsum('dbhij,dbhj->dbhi', s, r)
    return lax.scan(step, s0, scan_in)


def rwkv_post(y, r, k, v, gd, g_up, r_k, ln_g, ln_b):
    B, T, C = r.shape
    mu = jnp.mean(y, axis=-1, keepdims=True)
    var = jnp.mean(jnp.square(y - mu), axis=-1, keepdims=True)
    yn = ((y - mu) * lax.rsqrt(var + RW_LN_EPS)).reshape(B, T, C) * ln_g + ln_b
    hs = lambda t: t.reshape(B, T, RW_HEADS, RW_HEAD)
    bonus = jnp.sum(hs(r) * hs(k) * r_k, axis=-1, keepdims=True) * hs(v)
    g = jax.nn.sigmoid(gd) @ g_up
    return (yn.astype(r.dtype) + bonus.reshape(B, T, C)) * g


def merge_branches(o_a, o_b, z_gate, w_branch_a, w_branch_b, w_out):
    g_a, g_b = jnp.split(jax.nn.sigmoid(z_gate), N_BRANCH, axis=-1)
    return (g_a * (o_a @ w_branch_a) + g_b * (o_b @ w_branch_b)) @ w_out


def peer_ffn(h, peer_wq, peer_keys, peer_u, peer_v):
    B, T, D = h.shape
    tok = h.reshape(-1, PEER_CHUNK, D)

    def one_chunk(xc):
        q = (xc @ peer_wq).reshape(PEER_CHUNK, PEER_HEADS, 2, PEER_QDIM // 2)
        s = jnp.einsum('chpd,hpkd->chpk', q, peer_keys).astype(jnp.float32)
        sv, si = lax.top_k(s, PEER_TOPK)
        cand = (sv[..., 0, :, None] + sv[..., 1, None, :]).reshape(PEER_CHUNK, PEER_HEADS, PEER_TOPK * PEER_TOPK)
        cidx = (si[..., 0, :, None] * N_KEYS + si[..., 1, None, :]).reshape(PEER_CHUNK, PEER_HEADS, PEER_TOPK * PEER_TOPK)
        top_s, pos = lax.top_k(cand, PEER_TOPK)
        eidx = jnp.take_along_axis(cidx, pos, axis=-1)
        gate = jax.nn.softmax(top_s, axis=-1)
        act = jax.nn.gelu(jnp.einsum('chkd,cd->chk', peer_u[eidx], xc))
        return jnp.einsum('chk,chkd->cd', (gate * act).astype(xc.dtype), peer_v[eidx])

    return lax.map(one_chunk, tok).reshape(B, T, D)


def hybrid_layer(x, ctx, c, c_ctx, w_mod, b_mod, norm1_g, w_in, q_norm_g, k_norm_g, diff_lambda, diff_out_g,
                 rw_shift, rw_w0, rw_w_up, rw_a0, rw_a_up, rw_g_up, rw_k_k, rw_k_a, rw_r_k, rw_ln_g, rw_ln_b,
                 w_branch_a, w_branch_b, w_out, norm2_g, peer_wq, peer_keys, peer_u, peer_v, layer_idx, update_ctx):
    B, S, _ = x.shape
    lam_init = 0.8 - 0.6 * math.exp(-0.3 * layer_idx)
    mod_x = jnp.split((jax.nn.silu(c) @ w_mod + b_mod)[:, None, :], N_MOD, axis=-1)
    mod_c = jnp.split((jax.nn.silu(c_ctx) @ w_mod + b_mod)[None, None, :], N_MOD, axis=-1)
    zx = jnp.split(modulate(rmsnorm(x, norm1_g), mod_x[0], mod_x[1]) @ w_in, IN_SPLIT_IDX, axis=-1)
    zc = jnp.split(modulate(rmsnorm(ctx, norm1_g), mod_c[0], mod_c[1]) @ w_in, IN_SPLIT_IDX, axis=-1)

    qx, kx, vx = diff_heads(zx[0], zx[1], zx[2], q_norm_g, k_norm_g, axial_angles(S))
    qc, kc, vc = diff_heads(zc[0], zc[1], zc[2], q_norm_g, k_norm_g, None)
    lam = jnp.exp(jnp.sum(diff_lambda[0] * diff_lambda[1])) - jnp.exp(jnp.sum(diff_lambda[2] * diff_lambda[3])) + lam_init
    k_all = jnp.concatenate([kc, kx], axis=3)
    v_all = jnp.concatenate([vc, vx], axis=2)
    o_ax = diff_post(diff_attention(qx, k_all, v_all, lam), diff_out_g, lam_init)

    scan_c, aux_c = rwkv_prepare(zc[3], rw_shift, rw_w0, rw_w_up, rw_a0, rw_a_up, rw_k_k, rw_k_a)
    scan_x, aux_x = rwkv_prepare(zx[3], rw_shift, rw_w0, rw_w_up, rw_a0, rw_a_up, rw_k_k, rw_k_a)
    s0 = jnp.zeros((2, B, RW_HEADS, RW_HEAD, RW_HEAD), jnp.float32)
    s_ctx, y_c = wkv_scan(s0, scan_c)
    _, y_x = wkv_scan(s_ctx, scan_x)
    o_bx = rwkv_post(from_scan(y_x), *aux_x, rw_g_up, rw_r_k, rw_ln_g, rw_ln_b)

    x = x + mod_x[2] * merge_branches(o_ax, o_bx, zx[4], w_branch_a, w_branch_b, w_out)
    x = x + mod_x[5] * peer_ffn(modulate(rmsnorm(x, norm2_g), mod_x[3], mod_x[4]), peer_wq, peer_keys, peer_u, peer_v)

    if update_ctx:
        o_ac = diff_post(diff_attention(qc, kc, vc, lam), diff_out_g, lam_init)
        o_bc = rwkv_post(from_scan(y_c), *aux_c, rw_g_up, rw_r_k, rw_ln_g, rw_ln_b)
        ctx = ctx + mod_c[2] * merge_branches(o_ac, o_bc, zc[4], w_branch_a, w_branch_b, w_out)
        ctx = ctx + mod_c[5] * peer_ffn(modulate(rmsnorm(ctx, norm2_g), mod_c[3], mod_c[4]), peer_wq, peer_keys, peer_u, peer_v)
    return x, ctx


def setup_inputs(seed: int = 0) -> dict:
    key = jax.random.key(seed)
    ks = iter(jax.random.split(key, 40))
    nrm = lambda shape, s: jax.random.normal(next(ks), shape, jnp.float32) * s
    L, D = DEPTH, D_MODEL
    return {
        'x': nrm((BATCH, SEQ, D), 1.0),
        'c': nrm((BATCH, D), 1.0),
        'ctx': nrm((BATCH, CTX_LEN, D), 1.0),
        'c_ctx': nrm((D,), 1.0),
        'w_mod': nrm((L, D, N_MOD * D), D ** -0.5),
        'b_mod': nrm((L, N_MOD * D), 0.01),
        'norm1_g': 1.0 + nrm((L, D), 0.02),
        'w_in': nrm((L, D, IN_WIDTH), D ** -0.5),
        'q_norm_g': 1.0 + nrm((L, DA_QK), 0.02),
        'k_norm_g': 1.0 + nrm((L, DA_QK), 0.02),
        'diff_lambda': nrm((L, 4, DA_QK), 0.1),
        'diff_out_g': 1.0 + nrm((L, DA_V), 0.02),
        'rw_shift': jnp.array([0.25, 0.5, 0.25], jnp.float32)[None, :, None] + nrm((L, 3, RW_SHIFT_CH), 0.1),
        'rw_w0': -6.0 + 5.0 * jax.random.uniform(next(ks), (L, 2, RW_WIDTH), jnp.float32),
        'rw_w_up': nrm((L, 2, RW_W_LORA, RW_WIDTH), 0.1 * RW_W_LORA ** -0.5),
        'rw_a0': nrm((L, 2, RW_WIDTH), 0.5),
        'rw_a_up': nrm((L, 2, RW_A_LORA, RW_WIDTH), 0.1 * RW_A_LORA ** -0.5),
        'rw_g_up': nrm((L, RW_G_LORA, RW_WIDTH), RW_G_LORA ** -0.5),
        'rw_k_k': 0.85 + nrm((L, RW_WIDTH), 0.1),
        'rw_k_a': 1.0 + nrm((L, RW_WIDTH), 0.05),
        'rw_r_k': nrm((L, RW_HEADS, RW_HEAD), 0.1),
        'rw_ln_g': 1.0 + nrm((L, RW_WIDTH), 0.02),
        'rw_ln_b': nrm((L, RW_WIDTH), 0.01),
        'w_branch_a': nrm((L, DA_WIDTH, D), DA_WIDTH ** -0.5),
        'w_branch_b': nrm((L, RW_WIDTH, D), RW_WIDTH ** -0.5),
        'w_out': nrm((L, D, D), D ** -0.5),
        'norm2_g': 1.0 + nrm((L, D), 0.02),
        'peer_wq': nrm((L, D, PEER_HEADS * PEER_QDIM), D ** -0.5),
        'peer_keys': nrm((L, PEER_HEADS, 2, N_KEYS, PEER_QDIM // 2), (PEER_QDIM // 2) ** -0.5),
        'peer_u': nrm((L, N_EXPERTS, D), D ** -0.5),
        'peer_v': nrm((L, N_EXPERTS, D), PEER_HEADS ** -0.5),
    }


def reference(x, c, ctx, c_ctx, w_mod, b_mod, norm1_g, w_in, q_norm_g, k_norm_g, diff_lambda, diff_out_g,
              rw_shift, rw_w0, rw_w_up, rw_a0, rw_a_up, rw_g_up, rw_k_k, rw_k_a, rw_r_k, rw_ln_g, rw_ln_b,
              w_branch_a, w_branch_b, w_out, norm2_g, peer_wq, peer_keys, peer_u, peer_v):
    for l in range(DEPTH):
        x, ctx = hybrid_layer(
            x, ctx, c, c_ctx, w_mod[l], b_mod[l], norm1_g[l], w_in[l], q_norm_g[l], k_norm_g[l], diff_lambda[l],
            diff_out_g[l], rw_shift[l], rw_w0[l], rw_w_up[l], rw_a0[l], rw_a_up[l], rw_g_up[l], rw_k_k[l], rw_k_a[l],
            rw_r_k[l], rw_ln_g[l], rw_ln_b[l], w_branch_a[l], w_branch_b[l], w_out[l], norm2_g[l], peer_wq[l],
            peer_keys[l], peer_u[l], peer_v[l], layer_idx=l, update_ctx=l < DEPTH - 1)
    return x
```

```python
from contextlib import ExitStack

import numpy as np
import concourse.bass as bass
import concourse.mybir as mybir
from concourse.bass_utils import run_bass_kernel_spmd

F32 = mybir.dt.float32
BF16 = mybir.dt.bfloat16
I32 = mybir.dt.int32
U32 = mybir.dt.uint32
AF = mybir.ActivationFunctionType
ALU = mybir.AluOpType
AX = mybir.AxisListType


class Buf:
    __slots__ = ("w", "r", "name")

    def __init__(self, name=""):
        self.w = []
        self.r = []
        self.name = name


class KB:
    NS = 8
    ND = 40

    def __init__(self, nc):
        self.nc = nc
        self.es = ExitStack()
        self.engs = {"pe": nc.tensor, "dve": nc.vector, "act": nc.scalar,
                     "pool": nc.gpsimd, "sp": nc.sync}
        self.sems = {e: [self.es.enter_context(nc.semaphore(f"s_{e}_{i}"))
                         for i in range(self.NS)] for e in self.engs}
        self.cnt = {e: 0 for e in self.engs}
        self.seen = {e: {e2: -1 for e2 in self.engs} for e in self.engs}
        self.dsems = [self.es.enter_context(nc.semaphore(f"d_{i}")) for i in range(self.ND)]
        self.dtarget = [0] * self.ND
        self.dnext = 0
        self.dseen = {e: [0] * self.ND for e in self.engs}
        self.ninst = 0
        self.nwait = 0
        self.nw = {}
        self.nd = {}
        self.snap = {e: [] for e in self.engs}
        self.dsnap = {}

    def sb(self, name, shape, dt=F32):
        return self.es.enter_context(self.nc.sbuf_tensor(name, list(shape), dt))

    def ps(self, name, shape, dt=F32):
        return self.es.enter_context(self.nc.psum_tensor(name, list(shape), dt))

    def _need(self, eng, reads, writes, extra=()):
        best_c, best_d = {}, {}

        def add(tok, raw):
            if tok[0] == "c":
                if tok[1] == eng and not raw:
                    return
                if self.seen[eng][tok[1]] >= tok[2]:
                    return
                if best_c.get(tok[1], -1) < tok[2]:
                    best_c[tok[1]] = tok[2]
            else:
                if self.dseen[eng][tok[1]] >= tok[2]:
                    return
                if best_d.get(tok[1], 0) < tok[2]:
                    best_d[tok[1]] = tok[2]
        for t in extra:
            add(t, True)
        for b in reads:
            for t in b.w:
                add(t, True)
        for b in writes:
            for t in b.w:
                add(t, False)
            for t in b.r:
                add(t, False)
        toks = [("c", e2, i) for e2, i in best_c.items()] + [("d", si, tg) for si, tg in best_d.items()]
        out = []
        for t in toks:
            implied = False
            if t[0] == "c":
                for u in toks:
                    if u is t:
                        continue
                    sn = self.snap[u[1]][u[2]] if u[0] == "c" else self.dsnap[(u[1], u[2])]
                    if sn.get(t[1], -1) >= t[2]:
                        implied = True
                        break
            if not implied:
                out.append(t)
        return out

    def _learn(self, eng, tok):
        if tok[0] == "c":
            sn = self.snap[tok[1]][tok[2]]
            if self.seen[eng][tok[1]] < tok[2]:
                self.seen[eng][tok[1]] = tok[2]
        else:
            sn = self.dsnap[(tok[1], tok[2])]
            if self.dseen[eng][tok[1]] < tok[2]:
                self.dseen[eng][tok[1]] = tok[2]
        for e3, i3 in sn.items():
            if self.seen[eng][e3] < i3:
                self.seen[eng][e3] = i3

    def _semval(self, tok):
        if tok[0] == "c":
            return self.sems[tok[1]][tok[2] % self.NS], tok[2] // self.NS + 1
        return self.dsems[tok[1]], tok[2]

    def _emit(self, eng, toks, fn, fold=True):
        if not fold:
            for t in toks:
                s, v = self._semval(t)
                self.engs[eng].wait_ge(s, v)
                self.nw[eng] = self.nw.get(eng, 0) + 1
                self.nwait += 1
            inst = fn(self.engs[eng])
            for t in toks:
                self._learn(eng, t)
            return inst
        for t in toks[:-1]:
            s, v = self._semval(t)
            self.engs[eng].wait_ge(s, v)
            self.nw[eng] = self.nw.get(eng, 0) + 1
            self.nwait += 1
        inst = fn(self.engs[eng])
        if toks:
            s, v = self._semval(toks[-1])
            inst.wait_op(s, v, "sem-ge")
        for t in toks:
            self._learn(eng, t)
        return inst

    def _wait(self, eng, tok, raw=True):
        if tok[0] == "c":
            if (tok[1] == eng and not raw) or self.seen[eng][tok[1]] >= tok[2]:
                return
        elif self.dseen[eng][tok[1]] >= tok[2]:
            return
        s, v = self._semval(tok)
        self.engs[eng].wait_ge(s, v)
        self.nw[eng] = self.nw.get(eng, 0) + 1
        self.nwait += 1
        self._learn(eng, tok)

    @staticmethod
    def _dom(old, new):
        return old[0] == "c" and new[0] == "c" and old[1] == new[1]

    def _mark(self, tok, reads, writes):
        for b in reads:
            b.r = [t for t in b.r if not self._dom(t, tok)]
            b.r.append(tok)
        for b in writes:
            b.w = [t for t in b.w if not self._dom(t, tok)]
            b.w.append(tok)
            b.r = []

    def op(self, eng, fn, reads=(), writes=(), fold=True):
        toks = self._need(eng, reads, writes)
        inst = self._emit(eng, toks, fn, fold=fold)
        n = self.cnt[eng]
        self.cnt[eng] = n + 1
        inst.then_inc(self.sems[eng][n % self.NS], 1)
        self.snap[eng].append(dict(self.seen[eng]))
        tok = ("c", eng, n)
        for b in writes:
            b.w = [t for t in b.w if (t[0] == "c" and t[1] == eng)]
        self._mark(tok, reads, writes)
        self.ninst += 1
        return inst

    def dma(self, eng, out, in_, reads=(), writes=(), fn=None, **kw):
        si = self.dnext
        self.dnext = (si + 1) % self.ND
        extra = [("d", si, self.dtarget[si])] if self.dtarget[si] > 0 else []
        toks = self._need(eng, reads, writes, extra=extra)
        inst = self._emit(eng, toks, fn if fn is not None else (lambda e: e.dma_start(out=out, in_=in_, **kw)))
        self.dtarget[si] += 16
        inst.then_inc(self.dsems[si], 16)
        self.dsnap[(si, self.dtarget[si])] = dict(self.seen[eng])
        self._mark(("d", si, self.dtarget[si]), reads, writes)
        self.ninst += 1
        self.nd[eng] = self.nd.get(eng, 0) + 1
        return inst

    def barrier(self, bufs=()):
        for e in self.engs:
            for e2 in self.engs:
                if e2 != e and self.cnt[e2] > 0:
                    self._wait(e, ("c", e2, self.cnt[e2] - 1))
            for si in range(self.ND):
                if self.dtarget[si] > 0:
                    self._wait(e, ("d", si, self.dtarget[si]))
        for b in bufs:
            b.w = []
            b.r = []

    def finish(self):
        for e2 in self.engs:
            if e2 != "sp" and self.cnt[e2] > 0:
                self._wait("sp", ("c", e2, self.cnt[e2] - 1))
        for si in range(self.ND):
            if self.dtarget[si] > 0:
                self._wait("sp", ("d", si, self.dtarget[si]))


SCAN_BLK = 64
SCAN_T4_ENG = "dve"
SCAN_V_ENG = "pool"
SCAN_F32R = True
SCAN_T3B_ENG = "dve"
SCAN_DIAG_NOVB = False


def scan_consts_np():
    p = np.arange(128)
    d = p // 64
    blockones = (d[:, None] == d[None, :]).astype(np.float32)
    Cy = np.zeros((128, 192), np.float32)
    Cy[p, 63 + 64 * d] = 1.0
    return {"c_blockones": blockones, "c_Cy": Cy}


def build_scan(kb, consts, scd, vrow, yd, nsteps, S, Sb, y_from=0, per_step=None, n_psa=2):
    nc = kb.nc
    blockones, Cy = consts["blockones"], consts["Cy"]
    cb = consts["buf"]
    B = SCAN_BLK
    nblk = nsteps // B
    NVB = 4
    es = ExitStack()
    sbt = lambda n, s, dt=F32: es.enter_context(nc.sbuf_tensor("sc_" + n, list(s), dt))
    pst = lambda n, s, dt=F32: es.enter_context(nc.psum_tensor("sc_" + n, list(s), dt))
    SC = [sbt(f"sc{i}", [128, 5, 16, B]) for i in range(2)]
    SCb = [Buf() for _ in range(2)]
    VB = [sbt(f"vb{i}", [128, 1024]) for i in range(NVB)]
    VBb = [Buf() for _ in range(NVB)]
    FR = mybir.dt.float32r if SCAN_F32R else F32
    T1 = sbt("t1", [128, 1024], FR); T1b = Buf()
    T2 = sbt("t2", [128, 1024]); T2b = Buf()
    T3 = [sbt(f"t3{i}", [128, 1024]) for i in range(2)]; T3b = [Buf() for _ in range(2)]
    SP = [sbt(f"sp{i}", [128, 1024]) for i in range(2)]; SPb = [Buf() for _ in range(2)]
    T4 = [sbt(f"t4{i}", [128, 1024], FR) for i in range(2)]; T4b = [Buf() for _ in range(2)]
    YS = [sbt(f"ys{i}", [128, 1024]) for i in range(2)]; YSb = [Buf() for _ in range(2)]
    S2 = sbt("s2", [128, 1024]); S2b = Buf()
    Sb_h = [[Sb, Buf()], [S2b, Buf()]]
    SSt = [S, S2]
    assert nsteps % 2 == 0
    T1hb = [Buf(), Buf()]; T2hb = [Buf(), Buf()]
    T3hb = [[Buf(), Buf()], [Buf(), Buf()]]
    pSA = [pst(f"psa{i}", [128, 1024]) for i in range(n_psa)]
    pSAhb = [[Buf(), Buf()] for _ in range(n_psa)]
    pY = pst("py", [128, 1024]); pYb = Buf()
    v3 = lambda t: t[:].rearrange("p (h i) -> p h i", i=64)
    hv = lambda t, hf: t[:, hf * 512:(hf + 1) * 512].rearrange("p (h i) -> p h i", i=64)
    T4ENG = SCAN_T4_ENG
    r32 = lambda ap: ap
    if SCAN_F32R:
        bo_r = sbt("bo_r", [128, 128], FR); cy_r = sbt("cy_r", [128, 192], FR); crb = Buf()
        kb.op("dve", lambda e: e.tensor_copy(out=bo_r[:], in_=blockones[:]), reads=[cb], writes=[crb])
        kb.op("dve", lambda e: e.tensor_copy(out=cy_r[:], in_=Cy[:]), reads=[cb], writes=[crb])
        blockones, Cy, cb = bo_r, cy_r, crb

    def load_block(bi):
        k = bi % 2
        s0 = bi * B
        for d in range(2):
            kb.dma("sp", SC[k][d * 64:(d + 1) * 64], scd[d][:, :, :, s0:s0 + B], writes=[SCb[k]])

    def issue_vb(s):
        for d in range(2):
            kb.dma("act" if d else "sp", VB[s % NVB][d * 64:(d + 1) * 64, :], vrow(d, s).partition_broadcast(64),
                   writes=[VBb[s % NVB]])

    def emit_y(s):
        bi, tt = divmod(s, B)
        k = bi % 2
        st = SSt[(s + 1) % 2]
        stb = Sb_h[(s + 1) % 2]
        t4, t4b = T4[s % 2], T4b[s % 2]
        r_bc = SC[k][:, 4, :, tt:tt + 1].to_broadcast([128, 16, 64])
        kb.op(T4ENG, lambda e: e.tensor_tensor(out=v3(t4), in0=v3(st), in1=r_bc, op=ALU.mult),
              reads=[stb[0], stb[1], SCb[k]], writes=[t4b])
        for hf in range(2):
            kb.op("pe", lambda e: e.matmul(pY[:, hf * 512:(hf + 1) * 512], r32(Cy[:, 63 - tt:63 - tt + 128]),
                                           t4[:, hf * 512:(hf + 1) * 512], start=(tt == 0), stop=(tt == B - 1)),
                  reads=[t4b, cb], writes=[pYb])
        if tt == B - 1:
            ys = YS[bi % 2]
            kb.op("act", lambda e: e.copy(out=ys[:], in_=pY[:]), reads=[pYb], writes=[YSb[bi % 2]])
            s0 = bi * B
            for d in range(2):
                kb.dma("sp", yd[d][s0:s0 + B, :], ys[d * 64:(d + 1) * 64, :], reads=[YSb[bi % 2]])

    load_block(0)
    for s in range(min(NVB - 1, nsteps)):
        issue_vb(s)
    for s in range(nsteps):
        bi, tt = divmod(s, B)
        k = bi % 2
        sc = lambda q: SC[k][:, q, :, tt:tt + 1].to_broadcast([128, 16, 64])
        sch = lambda q, hf: SC[k][:, q, hf * 8:(hf + 1) * 8, tt:tt + 1].to_broadcast([128, 8, 64])
        cur, curb = SSt[s % 2], Sb_h[s % 2]
        nxt, nxtb = SSt[(s + 1) % 2], Sb_h[(s + 1) % 2]
        psa, psab = pSA[s % n_psa], pSAhb[s % n_psa]
        t3, t3b = T3[s % 2], T3b[s % 2]
        sp_, spb = SP[s % 2], SPb[s % 2]
        for hf in range(2):
            kb.op("dve", lambda e: e.tensor_tensor(out=hv(T1, hf), in0=hv(cur, hf), in1=sch(1, hf), op=ALU.mult),
                  reads=[curb[hf], SCb[k]], writes=[T1hb[hf]])
            kb.op("pe", lambda e: e.matmul(psa[:, hf * 512:(hf + 1) * 512], r32(blockones[:]),
                                           T1[:, hf * 512:(hf + 1) * 512], start=True, stop=True),
                  reads=[T1hb[hf], cb], writes=[psab[hf]])
        t3h = T3hb[s % 2]
        kb.op(SCAN_T3B_ENG, lambda e: e.tensor_tensor(out=hv(t3, 1), in0=hv(VB[s % NVB], 1), in1=sch(3, 1), op=ALU.mult),
              reads=[VBb[s % NVB], SCb[k]], writes=[t3h[1]])
        if s - 1 >= y_from:
            emit_y(s - 1)
        if tt == 0 and bi + 1 < nblk:
            load_block(bi + 1)
        if s + NVB - 1 < nsteps and not SCAN_DIAG_NOVB:
            issue_vb(s + NVB - 1)
        if per_step is not None:
            per_step(s)
        kb.op("pool", lambda e: e.tensor_tensor(out=hv(t3, 0), in0=hv(VB[s % NVB], 0), in1=sch(3, 0), op=ALU.mult),
              reads=[VBb[s % NVB], SCb[k]], writes=[t3h[0]])
        kb.op("pool", lambda e: e.tensor_tensor(out=v3(sp_), in0=v3(cur), in1=sc(0), op=ALU.mult),
              reads=[curb[0], curb[1], SCb[k]], writes=[spb])
        kb.op("pool", lambda e: e.tensor_tensor(out=sp_[:], in0=sp_[:], in1=t3[:], op=ALU.add),
              reads=[spb, t3h[0], t3h[1]], writes=[spb])
        for hf in range(2):
            kb.op("dve", lambda e: e.tensor_tensor(out=hv(T2, hf), in0=hv(psa, hf), in1=sch(2, hf), op=ALU.mult),
                  reads=[psab[hf], SCb[k]], writes=[T2hb[hf]])
        for hf in range(2):
            kb.op("dve", lambda e: e.tensor_tensor(out=nxt[:, hf * 512:(hf + 1) * 512], in0=sp_[:, hf * 512:(hf + 1) * 512],
                                                   in1=T2[:, hf * 512:(hf + 1) * 512], op=ALU.subtract),
                  reads=[spb, T2hb[hf]], writes=[nxtb[hf]])
    if nsteps - 1 >= y_from:
        emit_y(nsteps - 1)
    return es


D = 1024
NX = 4096
NC_ = 256
NT = NX + NC_
EPS = 1e-6
INW = 8448
LAM_INIT = 0.2
OVERLAP_ATTN = True
FORCE_NPSA1 = False
PEER_PIPELINE = True
ATTN_LOWFP = False


class Ring:
    def __init__(self, nc, es, name, shape, dt, n, psum=False):
        alloc = nc.psum_tensor if psum else nc.sbuf_tensor
        self.t = [es.enter_context(alloc(f"{name}{i}", list(shape), dt)) for i in range(n)]
        self.b = [Buf() for _ in range(n)]
        self.i = 0

    def next(self):
        i = self.i
        self.i = (i + 1) % len(self.t)
        return self.t[i], self.b[i]


def const_inputs_np():
    c = scan_consts_np()
    c["c_ident"] = np.eye(128, dtype=np.float32)
    c["c_J"] = np.eye(128, dtype=np.float32)[::-1].copy()
    c["c_ones"] = np.ones((128, 128), np.float32)
    P = np.zeros((128, 128), np.float32)
    for dp in range(128):
        d = dp % 64
        if (d % 32) // 16 == 0:
            P[dp + 16, dp] = -1.0
        else:
            P[dp - 16, dp] = 1.0
    c["c_ropeP"] = P
    t = np.arange(NX)
    row = (t // 64).astype(np.float32)
    col = (t % 64).astype(np.float32)
    inv = (np.float32(10000.0) ** (-np.arange(16, dtype=np.float32) / np.float32(16))).astype(np.float32)
    ang = np.stack([row[:, None] * inv, col[:, None] * inv], 1).astype(np.float32)
    cos = np.ones((128, NT), np.float32)
    sin = np.zeros((128, NT), np.float32)
    for dp in range(128):
        d = dp % 64
        a, f = d // 32, d % 16
        cos[dp, NC_:] = np.cos(ang[:, a, f])
        sin[dp, NC_:] = np.sin(ang[:, a, f])
    c["c_cos"] = cos
    c["c_sin"] = sin
    cw = np.zeros((128, 255), np.float32)
    cw[:, 127] = 1.0
    c["c_Cw"] = cw
    c["c_iota"] = np.tile(np.arange(256, dtype=np.float32)[None, :], (128, 1))
    return c


INPUT_SHAPES = {
    "xT": [D, NT], "xtok": [NX, D], "cT": [D, 2],
    "w_mod": [D, 6 * D], "b_mod": [6 * D], "norm1_g": [D], "w_in": [D, INW],
    "q_norm_g": [64], "k_norm_g": [64], "diff_lambda": [4, 64], "diff_out_g": [128],
    "rw_shift": [3, 3328], "rw_w0": [2, D], "rw_w_up": [2, 64, D], "rw_a0": [2, D], "rw_a_up": [2, 64, D],
    "rw_g_up": [128, D], "rw_k_k": [D], "rw_k_a": [D], "rw_r_k": [D], "rw_ln_g": [D], "rw_ln_b": [D],
    "w_branch_a": [D, D], "w_branch_b": [D, D], "w_out": [D, D], "norm2_g": [D],
    "peer_wq": [D, 2048], "peer_keysT": [16, 128, 128], "peer_u": [16384, D], "peer_v": [16384, D],
}


class Prog:
    def __init__(self, debug=False, upto="E", dbg_keys=()):
        self.debug = debug
        self.dbg_keys = set(dbg_keys)
        self.upto = upto
        nc = bass.Bass("TRN2", target_bir_lowering=False)
        self.nc = nc
        self.kb = KB(nc)
        self.I = {k: nc.dram_tensor(k, s, F32, kind="ExternalInput").ap() for k, s in INPUT_SHAPES.items()}
        self.cnp = const_inputs_np()
        for k, v in self.cnp.items():
            self.I[k] = nc.dram_tensor(k, list(v.shape), F32, kind="ExternalInput").ap()
        self.out = nc.dram_tensor("out", [NX, D], F32, kind="ExternalOutput").ap()
        dr = lambda n, s, dt=F32: nc.dram_tensor(
            n, s, dt, kind=("ExternalOutput" if (debug and n[2:] in self.dbg_keys) else "Internal")).ap()
        self.S = {
            "QT": dr("s_QT", [8, 128, NX], BF16), "KT": dr("s_KT", [8, 128, NT], BF16),
            "VA": dr("s_VA", [NT, D], BF16), "ZR": dr("s_ZR", [26, 128, NT]), "GA": dr("s_GA", [16, 128, NX]),
            "scd0": dr("s_scd0", [64, 5, 16, NT]), "scd1": dr("s_scd1", [64, 5, 16, NT]),
            "vtok": dr("s_vtok", [NT, D]), "yd0": dr("s_yd0", [NT, D]), "yd1": dr("s_yd1", [NT, D]),
            "G1": dr("s_G1", [8, 128, NX]), "G2": dr("s_G2", [8, 128, NX]),
            "OAT": dr("s_OAT", [8, 128, NX]), "X1": dr("s_X1", [NX, D]), "H2": dr("s_H2", [NX, D], BF16), "PVB": dr("s_PVB", [16384, D], BF16),
        }
        self.Sb = {k: Buf(k) for k in self.S}
        self.build()

    def build(self):
        kb, nc, I = self.kb, self.nc, self.I
        ges = kb.es
        g = {}
        self.g = g
        cb = Buf("consts")
        g["cb"] = cb
        for nm in ["ident", "J", "ones", "ropeP", "blockones", "Cy", "Cw", "iota"]:
            shp = list(self.cnp["c_" + nm].shape)
            g[nm] = kb.sb("g_" + nm, shp)
            kb.dma("sp", g[nm][:], I["c_" + nm], writes=[cb])
        self._gt = {"g_vec": [128, 128], "g_shv": [128, 128], "g_der": [128, 32], "g_modT": [128, 16, 2],
                    "g_gs1": [128, 8, 2], "g_mrow": [128, 4, 1024], "g_lam": [128, 4]}
        self._gt = {k: kb.sb(k, v) for k, v in self._gt.items()}
        self.phase0()
        skip = getattr(self, "skip", "")
        if self.upto >= "A" and "A" not in skip:
            self.phaseA()
        if self.upto >= "B" and "B" not in skip:
            self.phaseB()
        if self.upto >= "C":
            self.phaseC()
        if self.upto >= "D":
            self.phaseD()
        if self.upto >= "E":
            self.phaseE1()
        if self.upto >= "F":
            self.phaseE2()
        kb.finish()

    def phase0(self):
        kb, nc, I, g = self.kb, self.nc, self.I, self.g
        cb = g["cb"]
        es = ExitStack()
        sbt = lambda n, s, dt=F32: es.enter_context(nc.sbuf_tensor("p0_" + n, list(s), dt))
        pst = lambda n, s, dt=F32: es.enter_context(nc.psum_tensor("p0_" + n, list(s), dt))
        st1 = sbt("st1", [128, 128]); st1b = Buf()
        st2 = sbt("st2", [128, 128]); st2b = Buf()
        kb.op("dve", lambda e: e.memset(st1[:], 0.0), writes=[st1b])
        kb.op("dve", lambda e: e.memset(st2[:], 0.0), writes=[st2b])
        rows = {}
        r = 0

        def put(name, ap, nrows):
            nonlocal r
            kb.dma("sp", st1[r:r + nrows, :], ap, writes=[st1b])
            rows[name] = r
            r += nrows
        v8 = lambda ap: ap.rearrange("(r c) -> r c", c=128)
        put("norm1_g", v8(I["norm1_g"]), 8)
        put("k_k", v8(I["rw_k_k"]), 8)
        put("k_a", v8(I["rw_k_a"]), 8)
        put("r_k", v8(I["rw_r_k"]), 8)
        put("ln_g", v8(I["rw_ln_g"]), 8)
        put("ln_b", v8(I["rw_ln_b"]), 8)
        put("w0_0", v8(I["rw_w0"][0]), 8)
        put("w0_1", v8(I["rw_w0"][1]), 8)
        put("a0_0", v8(I["rw_a0"][0]), 8)
        put("a0_1", v8(I["rw_a0"][1]), 8)
        qg2 = I["q_norm_g"].rearrange("(o c) -> o c", o=1)
        kg2 = I["k_norm_g"].rearrange("(o c) -> o c", o=1)
        rows["qg"] = r
        kb.dma("sp", st1[r:r + 1, 0:64], qg2, writes=[st1b]); kb.dma("sp", st1[r:r + 1, 64:128], qg2, writes=[st1b]); r += 1
        rows["kg"] = r
        kb.dma("sp", st1[r:r + 1, 0:64], kg2, writes=[st1b]); kb.dma("sp", st1[r:r + 1, 64:128], kg2, writes=[st1b]); r += 1
        put("b_mod", I["b_mod"][0:2048].rearrange("(r c) -> r c", c=128), 16)
        assert r <= 128
        kb.dma("sp", st2[0:78, :], I["rw_shift"].rearrange("k (c p) -> (k c) p", p=128), writes=[st2b])
        self.rows = rows
        vec = self._gt["g_vec"]; vecb = Buf()
        shv = self._gt["g_shv"]; shvb = Buf()
        pT = pst("pT", [128, 128]); pTb = Buf()
        kb.op("pe", lambda e: e.transpose(pT[:], st1[:], g["ident"][:]), reads=[st1b, cb], writes=[pTb])
        kb.op("dve", lambda e: e.tensor_copy(out=vec[:], in_=pT[:]), reads=[pTb], writes=[vecb])
        kb.op("pe", lambda e: e.transpose(pT[:], st2[:], g["ident"][:]), reads=[st2b, cb], writes=[pTb])
        kb.op("dve", lambda e: e.tensor_copy(out=shv[:], in_=pT[:]), reads=[pTb], writes=[shvb])
        g["vec"], g["vecb"], g["shv"], g["shvb"] = vec, vecb, shv, shvb
        der = self._gt["g_der"]; derb = Buf()
        g["der"], g["derb"] = der, derb
        rk = rows["k_a"]
        kb.op("dve", lambda e: e.tensor_scalar(out=der[:, 0:8], in0=vec[:, rk:rk + 8], scalar1=-1.0, scalar2=1.0,
                                               op0=ALU.mult, op1=ALU.add), reads=[vecb], writes=[derb])
        kb.op("dve", lambda e: e.tensor_scalar(out=der[:, 8:9], in0=vec[:, rows["qg"]:rows["qg"] + 1], scalar1=0.125,
                                               scalar2=None, op0=ALU.mult), reads=[vecb], writes=[derb])
        cT = sbt("cT", [128, 8, 2]); cTb = Buf()
        kb.dma("sp", cT[:], I["cT"].rearrange("(c p) m -> p c m", p=128), writes=[cTb])
        sl = sbt("sl", [128, 8, 2]); slb = Buf()
        kb.op("act", lambda e: e.activation(out=sl[:], in_=cT[:], func=AF.Silu), reads=[cTb], writes=[slb])
        slbc = sbt("slbc", [128, 8, 128]); slbcb = Buf()
        for kc in range(8):
            kb.op("dve", lambda e: e.tensor_copy(out=slbc[:, kc, :], in_=sl[:, kc, 0:1].to_broadcast([128, 128])),
                  reads=[slb], writes=[slbcb])
        wmod = I["w_mod"].rearrange("(kc p) n -> p kc n", p=128)
        modT = self._gt["g_modT"]; modTb = Buf()
        g["modT"], g["modTb"] = modT, modTb
        wr = Ring(nc, es, "p0_w", [128, 8, 512], F32, 2)
        pM = pst("pM", [128, 32]); pMb = Buf()
        for c4 in range(4):
            wt, wtb = wr.next()
            kb.dma("sp", wt[:], wmod[:, :, c4 * 512:(c4 + 1) * 512], writes=[wtb])
            for j in range(4):
                cc = c4 * 4 + j
                for kc in range(8):
                    kb.op("pe", lambda e: e.matmul(pM[:, cc * 2:cc * 2 + 2], wt[:, kc, j * 128:(j + 1) * 128], sl[:, kc, :],
                                                   start=(kc == 0), stop=(kc == 7)), reads=[wtb, slb], writes=[pMb])
        rb = rows["b_mod"]
        kb.op("dve", lambda e: e.tensor_tensor(out=modT[:], in0=pM[:].rearrange("p (c m) -> p c m", m=2),
                                               in1=vec[:, rb:rb + 16].rearrange("p (c o) -> p c o", o=1).to_broadcast([128, 16, 2]),
                                               op=ALU.add), reads=[pMb, vecb], writes=[modTb])
        gs1 = self._gt["g_gs1"]; gs1b = Buf()
        g["gs1"], g["gs1b"] = gs1, gs1b
        rn = rows["norm1_g"]
        kb.op("dve", lambda e: e.tensor_scalar(out=gs1[:], in0=modT[:, 8:16, :], scalar1=1.0, scalar2=None, op0=ALU.add),
              reads=[modTb], writes=[gs1b])
        kb.op("dve", lambda e: e.tensor_tensor(out=gs1[:], in0=gs1[:],
                                               in1=vec[:, rn:rn + 8].rearrange("p (c o) -> p c o", o=1).to_broadcast([128, 8, 2]),
                                               op=ALU.mult), reads=[gs1b, vecb], writes=[gs1b])
        mrow = self._gt["g_mrow"]; mrowb = Buf()
        g["mrow"], g["mrowb"] = mrow, mrowb
        pR = Ring(nc, es, "p0_pR", [128, 512], F32, 2, psum=True)
        bb = Ring(nc, es, "p0_bb", [128, 512], F32, 2)
        for vi, v in enumerate([2, 3, 4, 5]):
            for hf in range(2):
                c0 = v * 1024 + hf * 512
                wt, wtb = wr.next()
                kb.dma("sp", wt[:], wmod[:, :, c0:c0 + 512], writes=[wtb])
                bt, btb = bb.next()
                kb.dma("sp", bt[:], I["b_mod"][c0:c0 + 512].partition_broadcast(128), writes=[btb])
                pr, prb = pR.next()
                for kc in range(8):
                    kb.op("pe", lambda e: e.matmul(pr[:], slbc[:, kc, :], wt[:, kc, :], start=(kc == 0), stop=(kc == 7)),
                          reads=[wtb, slbcb], writes=[prb])
                kb.op("dve", lambda e: e.tensor_tensor(out=mrow[:, vi, hf * 512:(hf + 1) * 512], in0=pr[:], in1=bt[:], op=ALU.add),
                      reads=[prb, btb], writes=[mrowb])
        n2 = sbt("n2", [128, 1024]); n2b = Buf()
        kb.dma("sp", n2[:], I["norm2_g"].partition_broadcast(128), writes=[n2b])
        kb.op("dve", lambda e: e.scalar_tensor_tensor(out=mrow[:, 2, :], in0=mrow[:, 2, :], scalar=1.0, in1=n2[:],
                                                      op0=ALU.add, op1=ALU.mult), reads=[mrowb, n2b], writes=[mrowb])
        dl = sbt("dl", [128, 256]); dlb = Buf()
        kb.dma("sp", dl[:], I["diff_lambda"].rearrange("a b -> (a b)").partition_broadcast(128), writes=[dlb])
        lam = self._gt["g_lam"]; lamb = Buf()
        g["lam"], g["lamb"] = lam, lamb
        pr_ = sbt("lpr", [128, 128]); prb_ = Buf()
        kb.op("dve", lambda e: e.tensor_tensor(out=pr_[:].rearrange("p (a c) -> p a c", a=2),
                                               in0=dl[:].rearrange("p (a t c) -> p a t c", a=2, t=2)[:, :, 0, :],
                                               in1=dl[:].rearrange("p (a t c) -> p a t c", a=2, t=2)[:, :, 1, :], op=ALU.mult),
              reads=[dlb], writes=[prb_])
        kb.op("dve", lambda e: e.tensor_reduce(out=lam[:, 0:2], in_=pr_[:].rearrange("p (a c) -> p a c", a=2), axis=AX.X, op=ALU.add),
              reads=[prb_], writes=[lamb])
        kb.op("act", lambda e: e.activation(out=lam[:, 0:2], in_=lam[:, 0:2], func=AF.Exp), reads=[lamb], writes=[lamb])
        kb.op("dve", lambda e: e.tensor_tensor(out=lam[:, 2:3], in0=lam[:, 0:1], in1=lam[:, 1:2], op=ALU.subtract),
              reads=[lamb], writes=[lamb])
        kb.op("dve", lambda e: e.tensor_scalar(out=lam[:, 3:4], in0=lam[:, 2:3], scalar1=LAM_INIT, scalar2=-1.0,
                                               op0=ALU.add, op1=ALU.mult), reads=[lamb], writes=[lamb])
        kb.barrier(list(self.Sb.values()))
        es.close()

    def phaseA(self):
        kb, nc, I, g, S, Sb = self.kb, self.nc, self.I, self.g, self.S, self.Sb
        cb, vec, vecb, der, derb, rows = g["cb"], g["vec"], g["vecb"], g["der"], g["derb"], self.rows
        gs1, gs1b, modT, modTb = g["gs1"], g["gs1b"], g["modT"], g["modTb"]
        es = ExitStack()
        sbt = lambda n, s, dt=F32: es.enter_context(nc.sbuf_tensor("pa_" + n, list(s), dt))
        pst = lambda n, s, dt=F32: es.enter_context(nc.psum_tensor("pa_" + n, list(s), dt))
        xTv = I["xT"].rearrange("(c p) t -> p c t", p=128)
        winv = I["w_in"].rearrange("(kc p) n -> p kc n", p=128)
        xr = Ring(nc, es, "pa_x", [128, 8, 512], F32, 2)
        sq = sbt("sq", [128, 8, 512]); sqb = Buf()
        hT = sbt("hT", [128, 8, 512], BF16); hTb = Buf()
        std = sbt("std", [128, 512]); stdb = Buf()
        rstd = sbt("rstd", [128, 512]); rstdb = Buf()
        cs = sbt("cs", [128, 2, 512]); csb = Buf()
        wk = Ring(nc, es, "pa_wk", [128, 512], F32, 12)
        wkb = Ring(nc, es, "pa_wkb", [128, 512], BF16, 4)
        wr = Ring(nc, es, "pa_w", [128, 8, 128], BF16, 4)
        wvr = Ring(nc, es, "pa_wv", [128, 8, 512], BF16, 2)
        pZ = Ring(nc, es, "pa_pZ", [128, 512], F32, 3, psum=True)
        pS = Ring(nc, es, "pa_pS", [128, 512], F32, 2, psum=True)
        pRr = Ring(nc, es, "pa_pR", [128, 512], F32, 2, psum=True)
        pN = pst("pN", [128, 512]); pNb = Buf()
        groups = [(0, 256, 1)] + [(256 + 512 * i, 512, 0) for i in range(8)]
        for (t0, n, m) in groups:
            xt, xtb = xr.next()
            kb.dma("sp", xt[:, :, :n], xTv[:, :, t0:t0 + n], writes=[xtb])
            kb.dma("sp", cs[:, 0, :n], I["c_cos"][:, t0:t0 + n], writes=[csb])
            kb.dma("sp", cs[:, 1, :n], I["c_sin"][:, t0:t0 + n], writes=[csb])
            kb.op("act", lambda e: e.activation(out=sq[:, :, :n], in_=xt[:, :, :n], func=AF.Square), reads=[xtb], writes=[sqb])
            for c in range(8):
                kb.op("pe", lambda e: e.matmul(pN[:, :n], g["ones"][:], sq[:, c, :n], start=(c == 0), stop=(c == 7)),
                      reads=[sqb, cb], writes=[pNb])
            kb.op("act", lambda e: e.activation(out=std[:, :n], in_=pN[:, :n], func=AF.Sqrt, bias=EPS, scale=1.0 / D),
                  reads=[pNb], writes=[stdb])
            kb.op("dve", lambda e: e.reciprocal(out=rstd[:, :n], in_=std[:, :n]), reads=[stdb], writes=[rstdb])
            for c in range(8):
                tm, tmb = wk.next()
                kb.op("dve", lambda e: e.tensor_tensor(out=tm[:, :n], in0=xt[:, c, :n], in1=rstd[:, :n], op=ALU.mult),
                      reads=[xtb, rstdb], writes=[tmb])
                kb.op("act", lambda e: e.activation(out=hT[:, c, :n], in_=tm[:, :n], func=AF.Identity,
                                                    scale=gs1[:, c, m:m + 1], bias=modT[:, c, m:m + 1]),
                      reads=[tmb, gs1b, modTb], writes=[hTb])

            def proj_fm(col0):
                wt, wtb = wr.next()
                kb.dma("pool", wt[:], winv[:, :, col0:col0 + 128], writes=[wtb])
                pz, pzb = pZ.next()
                for kc in range(8):
                    kb.op("pe", lambda e: e.matmul(pz[:, :n], wt[:, kc, :], hT[:, kc, :n], start=(kc == 0), stop=(kc == 7)),
                          reads=[wtb, hTb], writes=[pzb])
                return pz, pzb

            for cc in range(16):
                isq = cc < 8
                if isq and m == 1:
                    continue
                pz, pzb = proj_fm(cc * 128)
                gcol = der[:, 8:9] if isq else vec[:, rows["kg"]:rows["kg"] + 1]
                sqv, sqvb = wk.next()
                kb.op("act", lambda e: e.activation(out=sqv[:, :n], in_=pz[:, :n], func=AF.Square), reads=[pzb], writes=[sqvb])
                qg, qgb = wk.next()
                kb.op("act", lambda e: e.activation(out=qg[:, :n], in_=pz[:, :n], func=AF.Identity, scale=gcol),
                      reads=[pzb, derb, vecb], writes=[qgb])
                ps_, psb = pS.next()
                kb.op("pe", lambda e: e.matmul(ps_[:, :n], g["blockones"][:], sqv[:, :n], start=True, stop=True),
                      reads=[sqvb, cb], writes=[psb])
                pr_, prb = pRr.next()
                kb.op("pe", lambda e: e.matmul(pr_[:, :n], g["ropeP"][:], qg[:, :n], start=True, stop=True),
                      reads=[qgb, cb], writes=[prb])
                sd, sdb = wk.next()
                kb.op("act", lambda e: e.activation(out=sd[:, :n], in_=ps_[:, :n], func=AF.Sqrt, bias=EPS, scale=1.0 / 64),
                      reads=[psb], writes=[sdb])
                rs, rsb = wk.next()
                kb.op("dve", lambda e: e.reciprocal(out=rs[:, :n], in_=sd[:, :n]), reads=[sdb], writes=[rsb])
                t1, t1b = wk.next()
                kb.op("dve", lambda e: e.tensor_tensor(out=t1[:, :n], in0=qg[:, :n], in1=cs[:, 0, :n], op=ALU.mult),
                      reads=[qgb, csb], writes=[t1b])
                t2, t2b = wk.next()
                kb.op("dve", lambda e: e.tensor_tensor(out=t2[:, :n], in0=pr_[:, :n], in1=cs[:, 1, :n], op=ALU.mult),
                      reads=[prb, csb], writes=[t2b])
                kb.op("dve", lambda e: e.tensor_tensor(out=t1[:, :n], in0=t1[:, :n], in1=t2[:, :n], op=ALU.add),
                      reads=[t1b, t2b], writes=[t1b])
                qf, qfb = wkb.next()
                kb.op("dve", lambda e: e.tensor_tensor(out=qf[:, :n], in0=t1[:, :n], in1=rs[:, :n], op=ALU.mult),
                      reads=[t1b, rsb], writes=[qfb])
                if isq:
                    kb.dma("sp", S["QT"][cc, :, t0 - NC_:t0 - NC_ + n], qf[:, :n], reads=[qfb], writes=[Sb["QT"]])
                else:
                    kb.dma("sp", S["KT"][cc - 8, :, t0:t0 + n], qf[:, :n], reads=[qfb], writes=[Sb["KT"]])
            for hf in range(2):
                wv, wvb = wvr.next()
                kb.dma("pool", wv[:], winv[:, :, 2048 + hf * 512:2048 + (hf + 1) * 512], writes=[wvb])
                for ti in range(n // 128):
                    pz, pzb = pZ.next()
                    for kc in range(8):
                        kb.op("pe", lambda e: e.matmul(pz[:], hT[:, kc, ti * 128:(ti + 1) * 128], wv[:, kc, :],
                                                       start=(kc == 0), stop=(kc == 7)), reads=[wvb, hTb], writes=[pzb])
                    ob, obb = wkb.next()
                    kb.op("act", lambda e: e.copy(out=ob[:], in_=pz[:]), reads=[pzb], writes=[obb])
                    kb.dma("sp", S["VA"][t0 + ti * 128:t0 + (ti + 1) * 128, hf * 512:(hf + 1) * 512], ob[:],
                           reads=[obb], writes=[Sb["VA"]])
            for c in range(26):
                pz, pzb = proj_fm(3072 + c * 128)
                ob, obb = wk.next()
                kb.op("act", lambda e: e.copy(out=ob[:, :n], in_=pz[:, :n]), reads=[pzb], writes=[obb])
                kb.dma("sp", S["ZR"][c, :, t0:t0 + n], ob[:, :n], reads=[obb], writes=[Sb["ZR"]])
            if m == 0:
                for c in range(16):
                    pz, pzb = proj_fm(6400 + c * 128)
                    ob, obb = wk.next()
                    kb.op("act", lambda e: e.activation(out=ob[:, :n], in_=pz[:, :n], func=AF.Sigmoid), reads=[pzb], writes=[obb])
                    kb.dma("sp", S["GA"][c, :, t0 - NC_:t0 - NC_ + n], ob[:, :n], reads=[obb], writes=[Sb["GA"]])
        kb.barrier(list(self.Sb.values()))
        es.close()

    def phaseB(self):
        kb, nc, I, g, S, Sb = self.kb, self.nc, self.I, self.g, self.S, self.Sb
        cb, vec, vecb, der, derb, rows = g["cb"], g["vec"], g["vecb"], g["der"], g["derb"], self.rows
        shv, shvb = g["shv"], g["shvb"]
        es = ExitStack()
        sbt = lambda n, s, dt=F32: es.enter_context(nc.sbuf_tensor("pb_" + n, list(s), dt))
        n = 256
        zs = sbt("zs", [128, 26, n]); zsb = Buf()
        zring = Ring(nc, es, "pb_zr", [128, n + 2], F32, 3)
        tw = sbt("tw", [128, n]); twb = Buf()
        sg = sbt("sg", [128, n]); sgb = Buf()
        wk = Ring(nc, es, "pb_wk", [128, n], F32, 28)
        stgr = Ring(nc, es, "pb_stg", [128, 5, n], F32, 4)
        vtr = Ring(nc, es, "pb_vt", [128, 1024], F32, 2)
        pr = Ring(nc, es, "pb_p", [128, 512], F32, 4, psum=True)
        pbig = es.enter_context(nc.psum_tensor("pb_big", [128, 1024], F32)); pbigb = Buf()
        wup = sbt("wup", [64, 2, 1024]); wupb = Buf()
        aup = sbt("aup", [128, 2, 1024]); aupb = Buf()
        gup = sbt("gup", [128, 1024]); gupb = Buf()
        for d in range(2):
            kb.dma("sp", wup[:, d, :], I["rw_w_up"][d], writes=[wupb])
            kb.dma("sp", aup[64:128, d, :], I["rw_a_up"][d], writes=[aupb])
        kb.dma("sp", gup[:], I["rw_g_up"], writes=[gupb])
        col = lambda name, c: vec[:, rows[name] + c:rows[name] + c + 1]
        DEC = -float(np.exp(-0.5))
        groups = [(0, 0, NC_)] + [(NC_ + n * i, NC_, NT) for i in range(NX // n)]
        for (t0, s_lo, s_hi) in groups:
            isctx = t0 < NC_
            for c in range(26):
                zr, zrb = zring.next()
                lo = t0 - 1 if t0 > s_lo else t0
                hi = t0 + n + 1 if t0 + n < s_hi else t0 + n
                if t0 == s_lo:
                    kb.op("dve", lambda e: e.memset(zr[:, 0:1], 0.0), writes=[zrb])
                if t0 + n == s_hi:
                    kb.op("dve", lambda e: e.memset(zr[:, n + 1:n + 2], 0.0), writes=[zrb])
                kb.dma("sp", zr[:, lo - (t0 - 1):hi - (t0 - 1)], S["ZR"][c, :, lo:hi], reads=[Sb["ZR"]], writes=[zrb])
                w0_, w1_, w2_ = (shv[:, k * 26 + c:k * 26 + c + 1] for k in range(3))
                kb.op("dve", lambda e: e.tensor_scalar(out=zs[:, c, :], in0=zr[:, 1:n + 1], scalar1=w1_, scalar2=None, op0=ALU.mult),
                      reads=[zrb, shvb], writes=[zsb])
                kb.op("dve", lambda e: e.scalar_tensor_tensor(out=zs[:, c, :], in0=zr[:, 0:n], scalar=w0_, in1=zs[:, c, :],
                                                              op0=ALU.mult, op1=ALU.add), reads=[zrb, shvb, zsb], writes=[zsb])
                kb.op("dve", lambda e: e.scalar_tensor_tensor(out=zs[:, c, :], in0=zr[:, 2:n + 2], scalar=w2_, in1=zs[:, c, :],
                                                              op0=ALU.mult, op1=ALU.add), reads=[zrb, shvb, zsb], writes=[zsb])
            kb.op("act", lambda e: e.activation(out=tw[0:64, :], in_=zs[0:64, 24, :], func=AF.Tanh), reads=[zsb], writes=[twb])
            if not isctx:
                kb.op("act", lambda e: e.activation(out=sg[:], in_=zs[:, 25, :], func=AF.Sigmoid), reads=[zsb], writes=[sgb])
            s0 = [t0, 0 if isctx else NT - t0]
            for c in range(8):
                A = []
                STG = [stgr.next(), stgr.next()]
                for d in range(2):
                    pw, pwb = pr.next()
                    kb.op("pe", lambda e: e.matmul(pw[:, :n], wup[0:64, d, c * 128:(c + 1) * 128], tw[0:64, :], start=True, stop=True),
                          reads=[wupb, twb], writes=[pwb])
                    s1, s1b = wk.next()
                    kb.op("act", lambda e: e.activation(out=s1[:], in_=pw[:, :n], func=AF.Sigmoid, bias=col("w0_%d" % d, c)),
                          reads=[pwb, vecb], writes=[s1b])
                    stg, stgb = STG[d]
                    kb.op("act", lambda e: e.activation(out=(stg[:, 0, ::-1] if d else stg[:, 0, :]), in_=s1[:], func=AF.Exp, scale=DEC),
                          reads=[s1b], writes=[stgb])
                    pa, pab = pr.next()
                    kb.op("pe", lambda e: e.matmul(pa[:, :n], aup[64:128, d, c * 128:(c + 1) * 128], zs[64:128, 24, :], start=True, stop=True),
                          reads=[aupb, zsb], writes=[pab])
                    ad, adb = wk.next()
                    kb.op("act", lambda e: e.activation(out=ad[:], in_=pa[:, :n], func=AF.Sigmoid, bias=col("a0_%d" % d, c)),
                          reads=[pab, vecb], writes=[adb])
                    A.append((ad, adb))
                kq, kqb = wk.next()
                kb.op("act", lambda e: e.activation(out=kq[:], in_=zs[:, 8 + c, :], func=AF.Identity, scale=col("k_k", c)),
                      reads=[zsb, vecb], writes=[kqb])
                sqk, sqkb = wk.next()
                kb.op("act", lambda e: e.activation(out=sqk[:], in_=kq[:], func=AF.Square), reads=[kqb], writes=[sqkb])
                pk, pkb = pr.next()
                kb.op("pe", lambda e: e.matmul(pk[:, :n], g["blockones"][:], sqk[:], start=True, stop=True), reads=[sqkb, cb], writes=[pkb])
                sd, sdb = wk.next()
                kb.op("act", lambda e: e.activation(out=sd[:], in_=pk[:, :n], func=AF.Sqrt, bias=1e-12), reads=[pkb], writes=[sdb])
                rn, rnb = wk.next()
                kb.op("dve", lambda e: e.reciprocal(out=rn[:], in_=sd[:]), reads=[sdb], writes=[rnb])
                kk, kkb = wk.next()
                kb.op("dve", lambda e: e.tensor_tensor(out=kk[:], in0=kq[:], in1=rn[:], op=ALU.mult), reads=[kqb, rnb], writes=[kkb])
                kb.op("pool", lambda e: e.tensor_copy(out=STG[0][0][:, 1, :], in_=kk[:]), reads=[kkb], writes=[STG[0][1]])
                kb.op("pool", lambda e: e.tensor_copy(out=STG[1][0][:, 1, ::-1], in_=kk[:]), reads=[kkb], writes=[STG[1][1]])
                for d in range(2):
                    ad, adb = A[d]
                    tm, tmb = wk.next()
                    kb.op("dve", lambda e: e.tensor_scalar(out=tm[:], in0=ad[:], scalar1=col("k_a", c), scalar2=der[:, c:c + 1],
                                                           op0=ALU.mult, op1=ALU.add), reads=[adb, vecb, derb], writes=[tmb])
                    stg, stgb = STG[d]
                    kb.op("dve", lambda e: e.tensor_tensor(out=(stg[:, 3, ::-1] if d else stg[:, 3, :]), in0=tm[:], in1=zs[:, 8 + c, :], op=ALU.mult),
                          reads=[tmb, zsb], writes=[stgb])
                    kb.op("dve", lambda e: e.tensor_tensor(out=(stg[:, 2, ::-1] if d else stg[:, 2, :]), in0=kk[:], in1=ad[:], op=ALU.mult),
                          reads=[kkb, adb], writes=[stgb])
                kb.op("pool", lambda e: e.tensor_copy(out=STG[0][0][:, 4, :], in_=zs[:, c, :]), reads=[zsb], writes=[STG[0][1]])
                kb.op("pool", lambda e: e.tensor_copy(out=STG[1][0][:, 4, ::-1], in_=zs[:, c, :]), reads=[zsb], writes=[STG[1][1]])
                for d in range(2):
                    stg, stgb = STG[d]
                    dst = S["scd%d" % d]
                    for b in range(2):
                        kb.dma("pool" if (d + b) % 2 else "act", dst[:, :, 2 * c + b, s0[d]:s0[d] + n], stg[b * 64:(b + 1) * 64, :, :],
                               reads=[stgb], writes=[Sb["scd%d" % d]])
                if not isctx:
                    rk, rkb = wk.next()
                    kb.op("dve", lambda e: e.scalar_tensor_tensor(out=rk[:], in0=zs[:, c, :], scalar=col("r_k", c), in1=zs[:, 8 + c, :],
                                                                  op0=ALU.mult, op1=ALU.mult), reads=[zsb, vecb], writes=[rkb])
                    pb_, pbb = pr.next()
                    kb.op("pe", lambda e: e.matmul(pb_[:, :n], g["blockones"][:], rk[:], start=True, stop=True), reads=[rkb, cb], writes=[pbb])
                    bon, bonb = wk.next()
                    kb.op("dve", lambda e: e.tensor_tensor(out=bon[:], in0=pb_[:, :n], in1=zs[:, 16 + c, :], op=ALU.mult),
                          reads=[pbb, zsb], writes=[bonb])
                    pg, pgb = pr.next()
                    kb.op("pe", lambda e: e.matmul(pg[:, :n], gup[:, c * 128:(c + 1) * 128], sg[:], start=True, stop=True),
                          reads=[gupb, sgb], writes=[pgb])
                    g1, g1b = wk.next()
                    kb.op("dve", lambda e: e.tensor_scalar(out=g1[:], in0=pg[:, :n], scalar1=col("ln_g", c), scalar2=None, op0=ALU.mult),
                          reads=[pgb, vecb], writes=[g1b])
                    g2, g2b = wk.next()
                    kb.op("dve", lambda e: e.scalar_tensor_tensor(out=g2[:], in0=bon[:], scalar=col("ln_b", c), in1=pg[:, :n],
                                                                  op0=ALU.add, op1=ALU.mult), reads=[bonb, pgb, vecb], writes=[g2b])
                    kb.dma("act", S["G1"][c, :, t0 - NC_:t0 - NC_ + n], g1[:], reads=[g1b], writes=[Sb["G1"]])
                    kb.dma("pool", S["G2"][c, :, t0 - NC_:t0 - NC_ + n], g2[:], reads=[g2b], writes=[Sb["G2"]])
            for hf in range(n // 128):
                for c in range(8):
                    kb.op("pe", lambda e: e.transpose(pbig[:, c * 128:(c + 1) * 128], zs[:, 16 + c, hf * 128:(hf + 1) * 128], g["ident"][:]),
                          reads=[zsb, cb], writes=[pbigb])
                vt, vtb = vtr.next()
                kb.op("act", lambda e: e.copy(out=vt[:], in_=pbig[:]), reads=[pbigb], writes=[vtb])
                kb.dma("pool", S["vtok"][t0 + hf * 128:t0 + (hf + 1) * 128, :], vt[:], reads=[vtb], writes=[Sb["vtok"]])
        kb.barrier(list(self.Sb.values()))
        es.close()

    def phaseC(self):
        kb, nc, I, g, S, Sb = self.kb, self.nc, self.I, self.g, self.S, self.Sb
        es = ExitStack()
        St = es.enter_context(nc.sbuf_tensor("pc_S", [128, 1024], F32)); Stb = Buf()
        kb.op("dve", lambda e: e.memset(St[:], 0.0), writes=[Stb])
        consts = {"blockones": g["blockones"], "Cy": g["Cy"], "buf": g["cb"]}

        def vrow(d, s):
            if d == 0:
                t = s
            else:
                t = (NC_ - 1 - s) if s < NC_ else (NT + NC_ - 1 - s)
            return S["vtok"][t, :]
        nsteps = getattr(self, "scan_steps", NT)
        gen = None
        if OVERLAP_ATTN and self.upto >= "D":
            gen = self.attn_gen(es, overlap=True)
            self.overlapped = True
            next(gen, None)
        pvg = None
        if gen is not None and self.upto >= "F":
            pvg = self.pvb_gen(es)
            next(pvg, None)

        def per_step(s):
            if gen is not None:
                next(gen, None)
            if pvg is not None and s % 32 == 0:
                next(pvg, None)
        es2 = build_scan(kb, consts, [S["scd0"], S["scd1"]], vrow, [S["yd0"], S["yd1"]], nsteps, St, Stb, y_from=NC_,
                         per_step=per_step, n_psa=(1 if (gen is not None or FORCE_NPSA1) else 2))
        if gen is not None:
            for _ in gen:
                pass
        if pvg is not None:
            for _ in pvg:
                pass
        if self.debug:
            sd = nc.dram_tensor("dbg_S", [128, 1024], F32, kind="ExternalOutput").ap()
            kb.dma("sp", sd, St[:], reads=[Stb])
        kb.barrier(list(self.Sb.values()))
        es2.close()
        es.close()

    def attn_gen(self, es, overlap):
        kb, nc, I, g, S, Sb = self.kb, self.nc, self.I, self.g, self.S, self.Sb
        cb, lam, lamb = g["cb"], g["lam"], g["lamb"]
        sbt = lambda n, s, dt=F32: es.enter_context(nc.sbuf_tensor("pd_" + n, list(s), dt))
        NKT = NT // 128
        nb = 1 if overlap else 2
        KTr = Ring(nc, es, "pd_K", [128, NT], BF16, nb)
        QTr = Ring(nc, es, "pd_Q", [128, NX], BF16, nb)
        Vr = Ring(nc, es, "pd_V", [128, NKT, 128], BF16, nb)
        onesb = sbt("onesb", [128, 128], BF16); onesbb = Buf()
        kb.op("dve", lambda e: e.memset(onesb[:], 1.0), writes=[onesbb])
        og = sbt("og", [128, 1]); ogb = Buf()
        kb.dma("sp", og[:], I["diff_out_g"].rearrange("(p o) -> p o", o=1), writes=[ogb])
        kb.op("dve", lambda e: e.tensor_scalar(out=og[:], in0=og[:], scalar1=1.0 - LAM_INIT, scalar2=None, op0=ALU.mult),
              reads=[ogb], writes=[ogb])
        PTr = Ring(nc, es, "pd_PT", [128, 512], BF16, 4)
        wk = Ring(nc, es, "pd_wk", [128, 512], F32, 7)
        pS = Ring(nc, es, "pd_pS", [128, 512], F32, 2 if overlap else 3, psum=True)
        pO = es.enter_context(nc.psum_tensor("pd_pO", [128, 512], F32)); pOb = Buf()
        pZ = es.enter_context(nc.psum_tensor("pd_pZ", [128, 512], F32)); pZb = Buf()
        for h in range(8):
            kt_, ktb = KTr.next(); qt_, qtb = QTr.next(); vh, vhb = Vr.next()
            kb.dma("sp", kt_[:], S["KT"][h], reads=[Sb["KT"]], writes=[ktb])
            kb.dma("act", qt_[:], S["QT"][h], reads=[Sb["QT"]], writes=[qtb])
            vav = S["VA"][:, h * 128:(h + 1) * 128].rearrange("(kt p) c -> p kt c", p=128)
            for k2 in range(0, NKT, 2):
                kb.dma("sp" if (k2 // 2) % 2 else "act", vh[:, k2:k2 + 2, :], vav[:, k2:k2 + 2, :], reads=[Sb["VA"]], writes=[vhb])
            for qg in range(NX // 512):
                units = [(m, kt) for m in range(2) for kt in range(NKT)]

                def issue_scores(u):
                    m, kt = u
                    ps, psb = pS.next()
                    kb.op("pe", lambda e: e.matmul(ps[:], kt_[m * 64:(m + 1) * 64, kt * 128:(kt + 1) * 128],
                                                   qt_[m * 64:(m + 1) * 64, qg * 512:(qg + 1) * 512], start=True, stop=True),
                          reads=[ktb, qtb], writes=[psb])
                    return ps, psb
                nxt_s = issue_scores(units[0])
                maps = []
                for i, (m, kt) in enumerate(units):
                    ps, psb = nxt_s
                    if i + 1 < len(units):
                        nxt_s = issue_scores(units[i + 1])
                    pt, ptb = PTr.next()
                    kb.op("act", lambda e: e.activation(out=pt[:], in_=ps[:], func=AF.Exp), reads=[psb], writes=[ptb])
                    kb.op("pe", lambda e: e.matmul(pO[:], vh[:, kt, :], pt[:], start=(kt == 0), stop=(kt == NKT - 1)),
                          reads=[vhb, ptb], writes=[pOb])
                    kb.op("pe", lambda e: e.matmul(pZ[:], onesb[:], pt[:], start=(kt == 0), stop=(kt == NKT - 1)),
                          reads=[onesbb, ptb], writes=[pZb])
                    if kt == NKT - 1:
                        r_, rb_ = wk.next()
                        kb.op("dve", lambda e: e.reciprocal(out=r_[:], in_=pZ[:]), reads=[pZb], writes=[rb_])
                        a_, ab_ = wk.next()
                        kb.op("dve", lambda e: e.tensor_tensor(out=a_[:], in0=pO[:], in1=r_[:], op=ALU.mult), reads=[pOb, rb_], writes=[ab_])
                        maps.append((a_, ab_))
                    yield
                (a_, ab_), (b_, bb_) = maps
                kb.op("dve", lambda e: e.scalar_tensor_tensor(out=a_[:], in0=b_[:], scalar=lam[:, 3:4], in1=a_[:], op0=ALU.mult, op1=ALU.add),
                      reads=[ab_, bb_, lamb], writes=[ab_])
                sq, sqb = wk.next()
                kb.op("act", lambda e: e.activation(out=sq[:], in_=a_[:], func=AF.Square), reads=[ab_], writes=[sqb])
                pn, pnb = pS.next()
                kb.op("pe", lambda e: e.matmul(pn[:], g["ones"][:], sq[:], start=True, stop=True), reads=[sqb, cb], writes=[pnb])
                sd, sdb = wk.next()
                kb.op("act", lambda e: e.activation(out=sd[:], in_=pn[:], func=AF.Sqrt, bias=EPS, scale=1.0 / 128), reads=[pnb], writes=[sdb])
                kb.op("dve", lambda e: e.reciprocal(out=sd[:], in_=sd[:]), reads=[sdb], writes=[sdb])
                kb.op("dve", lambda e: e.tensor_tensor(out=a_[:], in0=a_[:], in1=sd[:], op=ALU.mult), reads=[ab_, sdb], writes=[ab_])
                o_, ob_ = wk.next()
                kb.op("act", lambda e: e.activation(out=o_[:], in_=a_[:], func=AF.Identity, scale=og[:, 0:1]), reads=[ab_, ogb], writes=[ob_])
                kb.dma("pool", S["OAT"][h, :, qg * 512:(qg + 1) * 512], o_[:], reads=[ob_], writes=[Sb["OAT"]])

    def pvb_gen(self, es):
        kb, nc, I, S, Sb = self.kb, self.nc, self.I, self.S, self.Sb
        fr = Ring(nc, es, "pv_f", [128, 1024], F32, 2)
        br = Ring(nc, es, "pv_b", [128, 1024], BF16, 2)
        yield
        for i in range(128):
            tf, tfb = fr.next()
            kb.dma("sp", tf[:], I["peer_v"][i * 128:(i + 1) * 128, :], writes=[tfb])
            tb, tbb = br.next()
            kb.op("act", lambda e: e.copy(out=tb[:], in_=tf[:]), reads=[tfb], writes=[tbb])
            kb.dma("act", S["PVB"][i * 128:(i + 1) * 128, :], tb[:], reads=[tbb], writes=[Sb["PVB"]])
            yield
        self.pvb_done = True

    def phaseD(self):
        if getattr(self, "overlapped", False):
            return
        kb = self.kb
        es = ExitStack()
        for _ in self.attn_gen(es, overlap=ATTN_LOWFP):
            pass
        kb.barrier(list(self.Sb.values()))
        es.close()

    def phaseE1(self):
        kb, nc, I, g, S, Sb = self.kb, self.nc, self.I, self.g, self.S, self.Sb
        cb, mrow, mrowb = g["cb"], g["mrow"], g["mrowb"]
        es = ExitStack()
        sbt = lambda n, s, dt=F32: es.enter_context(nc.sbuf_tensor("e1_" + n, list(s), dt))
        yr0 = Ring(nc, es, "e1_y0", [128, 1024], F32, 2)
        yr1 = Ring(nc, es, "e1_y1", [128, 1024], F32, 2)
        obT = sbt("obT", [128, 8, 512]); obTb = Buf()
        oaT = sbt("oaT", [128, 8, 512], BF16); oaTb = Buf()
        obH = sbt("obH", [128, 8, 512], BF16); obHb = Buf()
        mg = sbt("mg", [128, 8, 512], BF16); mgb = Buf()
        wr = Ring(nc, es, "e1_w", [128, 8, 128], BF16, 4)
        wo = Ring(nc, es, "e1_wo", [128, 8, 512], BF16, 2)
        gr = Ring(nc, es, "e1_g", [128, 512], F32, 6)
        wk = Ring(nc, es, "e1_wk", [128, 512], F32, 8)
        xr = Ring(nc, es, "e1_x", [128, 1024], F32, 2)
        x1r = Ring(nc, es, "e1_x1", [128, 1024], F32, 2)
        pT = Ring(nc, es, "e1_p", [128, 512], F32, 4, psum=True)
        pX = es.enter_context(nc.psum_tensor("e1_pX", [128, 1024], F32)); pXb = Buf()
        wav = I["w_branch_a"].rearrange("(kc p) n -> p kc n", p=128)
        wbv = I["w_branch_b"].rearrange("(kc p) n -> p kc n", p=128)
        wov = I["w_out"].rearrange("(kc p) n -> p kc n", p=128)
        for gi in range(NX // 512):
            t0 = gi * 512
            for ti in range(4):
                tt0 = t0 + ti * 128
                y0, y0b = yr0.next(); y1, y1b = yr1.next()
                kb.dma("sp", y0[:], S["yd0"][NC_ + tt0:NC_ + tt0 + 128, :], reads=[Sb["yd0"]], writes=[y0b])
                r0 = NT - 128 - tt0
                kb.dma("act", y1[:], S["yd1"][r0:r0 + 128, :], reads=[Sb["yd1"]], writes=[y1b])
                for c4 in range(2):
                    py, pyb = pT.next()
                    for cc in range(4):
                        c = c4 * 4 + cc
                        kb.op("pe", lambda e: e.matmul(py[:, cc * 128:(cc + 1) * 128], y0[:, c * 128:(c + 1) * 128], g["ident"][:],
                                                       start=True, stop=False), reads=[y0b, cb], writes=[pyb])
                        kb.op("pe", lambda e: e.matmul(py[:, cc * 128:(cc + 1) * 128], y1[:, c * 128:(c + 1) * 128], g["J"][:],
                                                       start=False, stop=True), reads=[y1b, cb], writes=[pyb])
                    kb.op("act", lambda e: e.copy(out=obT[:, c4 * 4:(c4 + 1) * 4, ti * 128:(ti + 1) * 128],
                                                  in_=py[:].rearrange("p (c t) -> p c t", t=128)), reads=[pyb], writes=[obTb])
            for c in range(8):
                pm, pmb = pT.next()
                kb.op("pe", lambda e: e.matmul(pm[:], g["blockones"][:], obT[:, c, :], start=True, stop=True), reads=[obTb, cb], writes=[pmb])
                yc, ycb = wk.next()
                kb.op("dve", lambda e: e.scalar_tensor_tensor(out=yc[:], in0=pm[:], scalar=-1.0 / 64, in1=obT[:, c, :], op0=ALU.mult, op1=ALU.add),
                      reads=[pmb, obTb], writes=[ycb])
                sq, sqb = wk.next()
                kb.op("act", lambda e: e.activation(out=sq[:], in_=yc[:], func=AF.Square), reads=[ycb], writes=[sqb])
                pv, pvb = pT.next()
                kb.op("pe", lambda e: e.matmul(pv[:], g["blockones"][:], sq[:], start=True, stop=True), reads=[sqb, cb], writes=[pvb])
                sd, sdb = wk.next()
                kb.op("act", lambda e: e.activation(out=sd[:], in_=pv[:], func=AF.Sqrt, bias=64e-5, scale=1.0 / 64), reads=[pvb], writes=[sdb])
                kb.op("dve", lambda e: e.reciprocal(out=sd[:], in_=sd[:]), reads=[sdb], writes=[sdb])
                kb.op("dve", lambda e: e.tensor_tensor(out=yc[:], in0=yc[:], in1=sd[:], op=ALU.mult), reads=[ycb, sdb], writes=[ycb])
                g1, g1b = gr.next(); g2, g2b = gr.next()
                kb.dma("sp", g1[:], S["G1"][c, :, t0:t0 + 512], reads=[Sb["G1"]], writes=[g1b])
                kb.dma("act", g2[:], S["G2"][c, :, t0:t0 + 512], reads=[Sb["G2"]], writes=[g2b])
                kb.op("dve", lambda e: e.tensor_tensor(out=yc[:], in0=yc[:], in1=g1[:], op=ALU.mult), reads=[ycb, g1b], writes=[ycb])
                kb.op("dve", lambda e: e.tensor_tensor(out=obH[:, c, :], in0=yc[:], in1=g2[:], op=ALU.add), reads=[ycb, g2b], writes=[obHb])
            kb.dma("pool", oaT[:], S["OAT"][:, :, t0:t0 + 512].rearrange("h p t -> p h t"), reads=[Sb["OAT"]], writes=[oaTb])
            for dc in range(8):
                wa, wab = wr.next()
                kb.dma("pool", wa[:], wav[:, :, dc * 128:(dc + 1) * 128], writes=[wab])
                pa, pab = pT.next()
                for kc in range(8):
                    kb.op("pe", lambda e: e.matmul(pa[:], wa[:, kc, :], oaT[:, kc, :], start=(kc == 0), stop=(kc == 7)), reads=[wab, oaTb], writes=[pab])
                wb_, wbb = wr.next()
                kb.dma("pool", wb_[:], wbv[:, :, dc * 128:(dc + 1) * 128], writes=[wbb])
                pb_, pbb = pT.next()
                for kc in range(8):
                    kb.op("pe", lambda e: e.matmul(pb_[:], wb_[:, kc, :], obH[:, kc, :], start=(kc == 0), stop=(kc == 7)), reads=[wbb, obHb], writes=[pbb])
                ga, gab = gr.next(); gb_, gbb = gr.next()
                kb.dma("sp", ga[:], S["GA"][dc, :, t0:t0 + 512], reads=[Sb["GA"]], writes=[gab])
                kb.dma("act", gb_[:], S["GA"][8 + dc, :, t0:t0 + 512], reads=[Sb["GA"]], writes=[gbb])
                m1, m1b = wk.next()
                kb.op("dve", lambda e: e.tensor_tensor(out=m1[:], in0=pa[:], in1=ga[:], op=ALU.mult), reads=[pab, gab], writes=[m1b])
                m2, m2b = wk.next()
                kb.op("dve", lambda e: e.tensor_tensor(out=m2[:], in0=pb_[:], in1=gb_[:], op=ALU.mult), reads=[pbb, gbb], writes=[m2b])
                kb.op("dve", lambda e: e.tensor_tensor(out=mg[:, dc, :], in0=m1[:], in1=m2[:], op=ALU.add), reads=[m1b, m2b], writes=[mgb])
            for hf in range(2):
                wt, wtb = wo.next()
                kb.dma("pool", wt[:], wov[:, :, hf * 512:(hf + 1) * 512], writes=[wtb])
                for ti in range(4):
                    tt0 = t0 + ti * 128
                    for kc in range(8):
                        kb.op("pe", lambda e: e.matmul(pX[:, 0:512], mg[:, kc, ti * 128:(ti + 1) * 128], wt[:, kc, :], start=(kc == 0), stop=(kc == 7)),
                              reads=[mgb, wtb], writes=[pXb])
                    xt, xtb = xr.next()
                    kb.dma("act", xt[:, 0:512], I["xtok"][tt0:tt0 + 128, hf * 512:(hf + 1) * 512], writes=[xtb])
                    x1, x1b = x1r.next()
                    kb.op("dve", lambda e: e.tensor_tensor(out=x1[:, 0:512], in0=pX[:, 0:512], in1=mrow[:, 0, hf * 512:(hf + 1) * 512], op=ALU.mult),
                          reads=[pXb, mrowb], writes=[x1b])
                    kb.op("dve", lambda e: e.tensor_tensor(out=x1[:, 0:512], in0=x1[:, 0:512], in1=xt[:, 0:512], op=ALU.add), reads=[x1b, xtb], writes=[x1b])
                    kb.dma("sp", S["X1"][tt0:tt0 + 128, hf * 512:(hf + 1) * 512], x1[:, 0:512], reads=[x1b], writes=[Sb["X1"]])
        kb.barrier(list(self.Sb.values()))
        es.close()

    def phaseE2(self):
        kb, nc, I, g, S, Sb = self.kb, self.nc, self.I, self.g, self.S, self.Sb
        cb, mrow, mrowb = g["cb"], g["mrow"], g["mrowb"]
        es = ExitStack()
        sbt = lambda n, s, dt=F32: es.enter_context(nc.sbuf_tensor("e2_" + n, list(s), dt))
        NEG = -1.0e30
        keysT = sbt("keysT", [128, 16, 128]); keysTb = Buf()
        kb.dma("sp", keysT[:], I["peer_keysT"].rearrange("j q k -> q j k"), writes=[keysTb])
        x1r = Ring(nc, es, "e2_x1", [128, 1024], F32, 3)
        h2r = Ring(nc, es, "e2_h2", [128, 1024], F32, 2)
        junk = sbt("junk", [128, 1024]); junkb = Buf()
        junkp = sbt("junkp", [128, 1024]); junkpb = Buf()
        h2T = sbt("h2T", [128, 8, 128]); h2Tb = Buf()
        qT = sbt("qT", [128, 16, 128]); qTb = Buf()
        wqr = Ring(nc, es, "e2_wq", [128, 8, 128], F32, 3)
        sc = sbt("sc", [128, 16, 128]); scb = Buf()
        sc2 = sbt("sc2", [128, 16, 128]); sc2b = Buf()
        sv = sbt("sv", [128, 16, 16]); svb = Buf()
        si = sbt("si", [128, 16, 16], U32); sib = Buf()
        sif = sbt("sif", [128, 16, 16]); sifb = Buf()
        cand = sbt("cand", [128, 8, 256]); candb = Buf()
        cand2 = sbt("cand2", [128, 8, 256]); cand2b = Buf()
        cidx = sbt("cidx", [128, 8, 256]); cidxb = Buf()
        tops = sbt("tops", [128, 8, 16]); topsb = Buf()
        pos = sbt("pos", [128, 8, 16], U32); posb = Buf()
        posf = sbt("posf", [128, 8, 16]); posfb = Buf()
        posg = sbt("posg", [128, 8, 16]); posgb = Buf()
        pa_ = sbt("pa_", [128, 8, 16], U32); pab_ = Buf()
        pb_ = sbt("pb_", [128, 8, 16], U32); pbb_ = Buf()
        sel1 = sbt("sel1", [128, 8, 16]); sel1b = Buf()
        sel2 = sbt("sel2", [128, 8, 16]); sel2b = Buf()
        eidx = sbt("eidx", [128, 128]); eidxb = Buf()
        eT = sbt("eT", [128, 128]); eTb = Buf()
        gate = sbt("gate", [128, 8, 16]); gateb = Buf()
        sm = sbt("sm", [128, 32]); smb = Buf()
        jk = sbt("jk", [128, 256]); jkb = Buf()
        gateT = [sbt(f"gateT{i}", [128, 128]) for i in range(2)]; gateTb = [Buf(), Buf()]
        idxT = [sbt(f"idxT{i}", [128, 128], U32) for i in range(2)]; idxTb = [Buf(), Buf()]
        dots = sbt("dots", [128, 128]); dotsb = Buf()
        coef = sbt("coef", [128, 128]); coefb = Buf()
        gw = Ring(nc, es, "e2_gw", [128, 128], F32, 4)
        ugr = Ring(nc, es, "e2_ug", [128, 1024], F32, 7)
        vgr = Ring(nc, es, "e2_vg", [128, 1024], BF16, 7)
        xbr = Ring(nc, es, "e2_xb", [128, 1024], BF16, 6)
        h2br = Ring(nc, es, "e2_h2b", [128, 1024], BF16, 2)
        Wr = Ring(nc, es, "e2_W", [128, 128], BF16, 4)
        outr = Ring(nc, es, "e2_o", [128, 1024], F32, 2)
        pT = Ring(nc, es, "e2_p", [128, 512], F32, 3, psum=True)
        pbig = es.enter_context(nc.psum_tensor("e2_pbig", [128, 1024], F32)); pbigb = Buf()
        pOut = es.enter_context(nc.psum_tensor("e2_pOut", [128, 1024], F32)); pOutb = Buf()
        for i in (range(128) if not getattr(self, "pvb_done", False) else ()):
            tf, tfb = ugr.next()
            kb.dma("sp" if i % 2 else "act", tf[:], I["peer_v"][i * 128:(i + 1) * 128, :], writes=[tfb])
            tb, tbb = vgr.next()
            kb.op("act" if i % 2 else "dve", lambda e: (e.copy(out=tb[:], in_=tf[:]) if i % 2 else e.tensor_copy(out=tb[:], in_=tf[:])),
                  reads=[tfb], writes=[tbb])
            kb.dma("pool", S["PVB"][i * 128:(i + 1) * 128, :], tb[:], reads=[tbb], writes=[Sb["PVB"]])
        wqv = I["peer_wq"].rearrange("(kc p) n -> p kc n", p=128)
        sv4 = sv[:].rearrange("p (h two) k -> p h two k", two=2)
        sif4 = sif[:].rearrange("p (h two) k -> p h two k", two=2)
        c4 = lambda t: t[:].rearrange("p h (a b) -> p h a b", b=16)
        res = {}

        def prep(ti):
            tt0 = ti * 128
            par = ti % 2
            x1, x1b = x1r.next()
            kb.dma("sp", x1[:], S["X1"][tt0:tt0 + 128, :], reads=[Sb["X1"]], writes=[x1b])
            kb.op("act", lambda e: e.activation(out=junkp[:], in_=x1[:], func=AF.Square, accum_out=sm[:, 0:1]), reads=[x1b], writes=[junkpb, smb], fold=False)
            kb.op("act", lambda e: e.activation(out=sm[:, 1:2], in_=sm[:, 0:1], func=AF.Sqrt, bias=EPS, scale=1.0 / D), reads=[smb], writes=[smb])
            kb.op("dve", lambda e: e.reciprocal(out=sm[:, 2:3], in_=sm[:, 1:2]), reads=[smb], writes=[smb])
            yield
            h2, h2b = h2r.next()
            kb.op("dve", lambda e: e.scalar_tensor_tensor(out=h2[:], in0=x1[:], scalar=sm[:, 2:3], in1=mrow[:, 2, :], op0=ALU.mult, op1=ALU.mult),
                  reads=[x1b, smb, mrowb], writes=[h2b])
            yield
            kb.op("dve", lambda e: e.tensor_tensor(out=h2[:], in0=h2[:], in1=mrow[:, 1, :], op=ALU.add), reads=[h2b, mrowb], writes=[h2b])
            yield
            h2bf, h2bfb = h2br.next()
            kb.op("act", lambda e: e.copy(out=h2bf[:], in_=h2[:]), reads=[h2b], writes=[h2bfb])
            kb.dma("act", S["H2"][tt0:tt0 + 128, :], h2bf[:], reads=[h2bfb], writes=[Sb["H2"]])
            for c in range(8):
                kb.op("pe", lambda e: e.transpose(pbig[:, c * 128:(c + 1) * 128], h2[:, c * 128:(c + 1) * 128], g["ident"][:]),
                      reads=[h2b, cb], writes=[pbigb])
            kb.op("act", lambda e: e.copy(out=h2T[:], in_=pbig[:].rearrange("p (c t) -> p c t", t=128)), reads=[pbigb], writes=[h2Tb])
            yield
            for j4 in range(4):
                pq, pqb = pT.next()
                for jj in range(4):
                    j = j4 * 4 + jj
                    wq, wqb = wqr.next()
                    kb.dma("sp" if j % 2 else "act", wq[:], wqv[:, :, j * 128:(j + 1) * 128], writes=[wqb])
                    for kc in range(8):
                        kb.op("pe", lambda e: e.matmul(pq[:, jj * 128:(jj + 1) * 128], wq[:, kc, :], h2T[:, kc, :], start=(kc == 0), stop=(kc == 7)),
                              reads=[wqb, h2Tb], writes=[pqb])
                    yield
                kb.op("act", lambda e: e.copy(out=qT[:, j4 * 4:(j4 + 1) * 4, :], in_=pq[:].rearrange("p (c t) -> p c t", t=128)), reads=[pqb], writes=[qTb])
            for j4 in range(4):
                psc, pscb = pT.next()
                for jj in range(4):
                    j = j4 * 4 + jj
                    kb.op("pe", lambda e: e.matmul(psc[:, jj * 128:(jj + 1) * 128], qT[:, j, :], keysT[:, j, :], start=True, stop=True),
                          reads=[qTb, keysTb], writes=[pscb])
                kb.op("act", lambda e: e.copy(out=sc[:, j4 * 4:(j4 + 1) * 4, :], in_=psc[:].rearrange("p (c t) -> p c t", t=128)), reads=[pscb], writes=[scb])
                yield
            for j in range(16):
                kb.op("dve", lambda e: e.max(out=sv[:, j, 0:8], in_=sc[:, j, :]), reads=[scb], writes=[svb])
                kb.op("dve", lambda e: e.max_index(out=si[:, j, 0:8], in_max=sv[:, j, 0:8], in_values=sc[:, j, :]), reads=[scb, svb], writes=[sib])
                yield
                kb.op("dve", lambda e: e.match_replace(out=sc2[:, j, :], in_to_replace=sv[:, j, 0:8], in_values=sc[:, j, :], imm_value=NEG),
                      reads=[scb, svb], writes=[sc2b])
                kb.op("dve", lambda e: e.max(out=sv[:, j, 8:16], in_=sc2[:, j, :]), reads=[sc2b], writes=[svb])
                yield
                kb.op("dve", lambda e: e.max_index(out=si[:, j, 8:16], in_max=sv[:, j, 8:16], in_values=sc2[:, j, :]), reads=[sc2b, svb], writes=[sib])
                yield
            kb.op("dve", lambda e: e.tensor_copy(out=sif[:], in_=si[:]), reads=[sib], writes=[sifb])
            kb.op("dve", lambda e: e.tensor_tensor(out=c4(cand), in0=sv4[:, :, 0, :].unsqueeze(3).to_broadcast([128, 8, 16, 16]),
                                                   in1=sv4[:, :, 1, :].unsqueeze(2).to_broadcast([128, 8, 16, 16]), op=ALU.add),
                  reads=[svb], writes=[candb])
            yield
            kb.op("dve", lambda e: e.tensor_scalar(out=sif4[:, :, 0, :], in0=sif4[:, :, 0, :], scalar1=128.0, scalar2=None, op0=ALU.mult),
                  reads=[sifb], writes=[sifb])
            kb.op("dve", lambda e: e.tensor_tensor(out=c4(cidx), in0=sif4[:, :, 0, :].unsqueeze(3).to_broadcast([128, 8, 16, 16]),
                                                   in1=sif4[:, :, 1, :].unsqueeze(2).to_broadcast([128, 8, 16, 16]), op=ALU.add),
                  reads=[sifb], writes=[cidxb])
            yield
            for h in range(8):
                kb.op("dve", lambda e: e.max(out=tops[:, h, 0:8], in_=cand[:, h, :]), reads=[candb], writes=[topsb])
                kb.op("dve", lambda e: e.max_index(out=pos[:, h, 0:8], in_max=tops[:, h, 0:8], in_values=cand[:, h, :]), reads=[candb, topsb], writes=[posb])
                yield
                kb.op("dve", lambda e: e.match_replace(out=cand2[:, h, :], in_to_replace=tops[:, h, 0:8], in_values=cand[:, h, :], imm_value=NEG),
                      reads=[candb, topsb], writes=[cand2b])
                kb.op("dve", lambda e: e.max(out=tops[:, h, 8:16], in_=cand2[:, h, :]), reads=[cand2b], writes=[topsb])
                yield
                kb.op("dve", lambda e: e.max_index(out=pos[:, h, 8:16], in_max=tops[:, h, 8:16], in_values=cand2[:, h, :]), reads=[cand2b, topsb], writes=[posb])
                yield
            kb.op("dve", lambda e: e.tensor_scalar(out=pa_[:], in0=pos[:], scalar1=4, scalar2=None, op0=ALU.logical_shift_right), reads=[posb], writes=[pab_])
            kb.op("dve", lambda e: e.tensor_scalar(out=pb_[:], in0=pos[:], scalar1=15, scalar2=None, op0=ALU.bitwise_and), reads=[posb], writes=[pbb_])
            kb.op("dve", lambda e: e.tensor_copy(out=posf[:], in_=pa_[:]), reads=[pab_], writes=[posfb])
            kb.op("dve", lambda e: e.tensor_copy(out=posg[:], in_=pb_[:]), reads=[pbb_], writes=[posgb])
            yield
            i16 = g["iota"][:, 0:16].unsqueeze(1).unsqueeze(1).to_broadcast([128, 8, 16, 16])
            for half, pf, pfb, acc, accb in ((0, posf, posfb, sel1, sel1b), (1, posg, posgb, sel2, sel2b)):
                kb.op("dve", lambda e: e.tensor_tensor(out=c4(cand2), in0=i16, in1=pf[:].unsqueeze(3).to_broadcast([128, 8, 16, 16]), op=ALU.is_equal),
                      reads=[pfb, cb], writes=[cand2b])
                yield
                kb.op("dve", lambda e: e.tensor_tensor(out=c4(cand2), in0=c4(cand2), in1=sif4[:, :, half, :].unsqueeze(2).to_broadcast([128, 8, 16, 16]), op=ALU.mult),
                      reads=[cand2b, sifb], writes=[cand2b])
                yield
                kb.op("dve", lambda e: e.tensor_reduce(out=acc[:], in_=c4(cand2), axis=AX.X, op=ALU.add), reads=[cand2b], writes=[accb])
                yield
            kb.op("dve", lambda e: e.tensor_tensor(out=eidx[:].rearrange("p (h k) -> p h k", k=16), in0=sel1[:], in1=sel2[:], op=ALU.add),
                  reads=[sel1b, sel2b], writes=[eidxb])
            yield
            kb.op("dve", lambda e: e.tensor_scalar(out=sm[:, 8:16], in0=tops[:, :, 0], scalar1=-1.0, scalar2=None, op0=ALU.mult), reads=[topsb], writes=[smb])
            for h in range(8):
                kb.op("act", lambda e: e.activation(out=gate[:, h, :], in_=tops[:, h, :], func=AF.Exp, bias=sm[:, 8 + h:9 + h], accum_out=sm[:, 16 + h:17 + h]),
                      reads=[topsb, smb], writes=[gateb, smb], fold=False)
            yield
            kb.op("dve", lambda e: e.reciprocal(out=sm[:, 24:32], in_=sm[:, 16:24]), reads=[smb], writes=[smb])
            kb.op("dve", lambda e: e.tensor_tensor(out=gate[:], in0=gate[:], in1=sm[:, 24:32].unsqueeze(2).to_broadcast([128, 8, 16]), op=ALU.mult),
                  reads=[gateb, smb], writes=[gateb])
            yield
            pg, pgb = pT.next()
            kb.op("pe", lambda e: e.transpose(pg[:, 0:128], gate[:].rearrange("p h k -> p (h k)"), g["ident"][:]), reads=[gateb, cb], writes=[pgb])
            kb.op("pe", lambda e: e.transpose(pg[:, 128:256], eidx[:], g["ident"][:]), reads=[eidxb, cb], writes=[pgb])
            kb.op("act", lambda e: e.copy(out=gateT[par][:], in_=pg[:, 0:128]), reads=[pgb], writes=[gateTb[par]])
            kb.op("act", lambda e: e.copy(out=eT[:], in_=pg[:, 128:256]), reads=[pgb], writes=[eTb])
            kb.op("dve", lambda e: e.tensor_copy(out=idxT[par][:], in_=eT[:]), reads=[eTb], writes=[idxTb[par]])
            res[ti] = (x1, x1b)
            yield

        def drain(gen):
            if gen is not None:
                for _ in gen:
                    pass

        def step(gen, n):
            if gen is not None:
                for _ in range(n):
                    next(gen, None)

        NTILE = NX // 128
        drain(prep(0))
        for ti in range(NTILE):
            tt0 = ti * 128
            par = ti % 2
            x1, x1b = res.pop(ti)
            idxT_, idxTb_, gateT_, gateTb_ = idxT[par], idxTb[par], gateT[par], gateTb[par]
            gen = prep(ti + 1) if (ti + 1 < NTILE and PEER_PIPELINE) else None
            for tok in range(128):
                ug, ugb = ugr.next()
                kb.dma("pool", None, None, reads=[idxTb_], writes=[ugb],
                       fn=lambda e: e.indirect_dma_start(out=ug[:], out_offset=None, in_=I["peer_u"],
                                                         in_offset=bass.IndirectOffsetOnAxis(ap=idxT_[:, tok:tok + 1], axis=0)))
                xb, xbb = xbr.next()
                kb.dma("sp" if tok % 2 else "act", xb[:], S["H2"][tt0 + tok, :].partition_broadcast(128), reads=[Sb["H2"]], writes=[xbb])
                kb.op("dve", lambda e: e.scalar_tensor_tensor(out=junk[:], in0=ug[:], scalar=1.0, in1=xb[:], op0=ALU.mult, op1=ALU.mult,
                                                              accum_out=dots[:, tok:tok + 1]), reads=[ugb, xbb], writes=[junkb, dotsb], fold=False)
                step(gen, 4)
            t1, t1b = gw.next(); t2, t2b = gw.next()
            kb.op("dve", lambda e: e.tensor_tensor(out=t1[:], in0=dots[:], in1=dots[:], op=ALU.mult), reads=[dotsb], writes=[t1b])
            kb.op("dve", lambda e: e.tensor_scalar(out=t1[:], in0=t1[:], scalar1=0.044715, scalar2=1.0, op0=ALU.mult, op1=ALU.add), reads=[t1b], writes=[t1b])
            kb.op("dve", lambda e: e.tensor_tensor(out=t1[:], in0=t1[:], in1=dots[:], op=ALU.mult), reads=[t1b, dotsb], writes=[t1b])
            kb.op("act", lambda e: e.activation(out=t2[:], in_=t1[:], func=AF.Tanh, scale=0.7978845608028654), reads=[t1b], writes=[t2b])
            kb.op("dve", lambda e: e.tensor_scalar(out=t2[:], in0=t2[:], scalar1=1.0, scalar2=0.5, op0=ALU.add, op1=ALU.mult), reads=[t2b], writes=[t2b])
            kb.op("dve", lambda e: e.tensor_tensor(out=t2[:], in0=t2[:], in1=dots[:], op=ALU.mult), reads=[t2b, dotsb], writes=[t2b])
            kb.op("dve", lambda e: e.tensor_tensor(out=coef[:], in0=t2[:], in1=gateT_[:], op=ALU.mult), reads=[t2b, gateTb_], writes=[coefb])
            for tok in range(128):
                vg, vgb = vgr.next()
                kb.dma("pool", None, None, reads=[idxTb_, Sb["PVB"]], writes=[vgb],
                       fn=lambda e: e.indirect_dma_start(out=vg[:], out_offset=None, in_=S["PVB"],
                                                         in_offset=bass.IndirectOffsetOnAxis(ap=idxT_[:, tok:tok + 1], axis=0)))
                W, Wb = Wr.next()
                kb.op("act", lambda e: e.activation(out=W[:], in_=g["Cw"][:, 127 - tok:255 - tok], func=AF.Identity, scale=coef[:, tok:tok + 1]),
                      reads=[coefb, cb], writes=[Wb])
                for hf in range(2):
                    kb.op("pe", lambda e: e.matmul(pOut[:, hf * 512:(hf + 1) * 512], W[:], vg[:, hf * 512:(hf + 1) * 512],
                                                   start=(tok == 0), stop=(tok == 127)), reads=[Wb, vgb], writes=[pOutb])
                pass
            ot, otb = outr.next()
            kb.op("dve", lambda e: e.tensor_tensor(out=ot[:], in0=pOut[:], in1=mrow[:, 3, :], op=ALU.mult), reads=[pOutb, mrowb], writes=[otb])
            kb.op("dve", lambda e: e.tensor_tensor(out=ot[:], in0=ot[:], in1=x1[:], op=ALU.add), reads=[otb, x1b], writes=[otb])
            kb.dma("sp", self.out[tt0:tt0 + 128, :], ot[:], reads=[otb])
            drain(gen)
        kb.barrier(list(self.Sb.values()))
        es.close()

_W_KEYS = ["w_mod", "b_mod", "norm1_g", "w_in", "q_norm_g", "k_norm_g", "diff_lambda", "diff_out_g", "rw_shift",
           "rw_w0", "rw_w_up", "rw_a0", "rw_a_up", "rw_g_up", "rw_k_k", "rw_k_a", "rw_ln_g", "rw_ln_b",
           "w_branch_a", "w_branch_b", "w_out", "norm2_g", "peer_wq", "peer_u", "peer_v"]


def prep_shared(inp, consts):
    d = {k: np.ascontiguousarray(np.asarray(inp[k], np.float32)[0]) for k in _W_KEYS}
    d["rw_r_k"] = np.ascontiguousarray(np.asarray(inp["rw_r_k"], np.float32)[0].reshape(D))
    d["peer_keysT"] = np.ascontiguousarray(np.asarray(inp["peer_keys"], np.float32)[0].transpose(0, 1, 3, 2).reshape(16, 128, 128))
    d.update(consts)
    return d


def prep_core(inp, b, shared):
    d = dict(shared)
    x = np.asarray(inp["x"], np.float32)[b]
    ctx = np.asarray(inp["ctx"], np.float32)[b]
    d["xT"] = np.ascontiguousarray(np.concatenate([ctx, x], 0).T)
    d["xtok"] = np.ascontiguousarray(x)
    d["cT"] = np.ascontiguousarray(np.stack([np.asarray(inp["c"], np.float32)[b], np.asarray(inp["c_ctx"], np.float32)], 1))
    return d


_PROG = None


def kernel(**inputs):
    global _PROG
    if _PROG is None:
        _PROG = Prog(debug=False, upto="F")
    shared = prep_shared(inputs, _PROG.cnp)
    in_maps = [prep_core(inputs, b, shared) for b in range(8)]
    res = run_bass_kernel_spmd(_PROG.nc, in_maps, core_ids=list(range(8)))
    return np.stack([np.asarray(r["out"], np.float32) for r in res.results], 0)
```

```python
from contextlib import ExitStack

import numpy as np
import concourse.bass as bass
import concourse.mybir as mybir
from concourse.bass_utils import run_bass_kernel_spmd

F32 = mybir.dt.float32
BF16 = mybir.dt.bfloat16
I32 = mybir.dt.int32
U32 = mybir.dt.uint32
AF = mybir.ActivationFunctionType
ALU = mybir.AluOpType
AX = mybir.AxisListType


class Buf:
    __slots__ = ("w", "r", "name")

    def __init__(self, name=""):
        self.w = []
        self.r = []
        self.name = name


class KB:
    NS = 8
    ND = 40

    def __init__(self, nc):
        self.nc = nc
        self.es = ExitStack()
        self.engs = {"pe": nc.tensor, "dve": nc.vector, "act": nc.scalar,
                     "pool": nc.gpsimd, "sp": nc.sync}
        self.sems = {e: [self.es.enter_context(nc.semaphore(f"s_{e}_{i}"))
                         for i in range(self.NS)] for e in self.engs}
        self.cnt = {e: 0 for e in self.engs}
        self.seen = {e: {e2: -1 for e2 in self.engs} for e in self.engs}
        self.dsems = [self.es.enter_context(nc.semaphore(f"d_{i}")) for i in range(self.ND)]
        self.dtarget = [0] * self.ND
        self.dnext = 0
        self.dseen = {e: [0] * self.ND for e in self.engs}
        self.ninst = 0
        self.nwait = 0
        self.nw = {}
        self.nd = {}
        self.snap = {e: [] for e in self.engs}
        self.dsnap = {}

    def sb(self, name, shape, dt=F32):
        return self.es.enter_context(self.nc.sbuf_tensor(name, list(shape), dt))

    def ps(self, name, shape, dt=F32):
        return self.es.enter_context(self.nc.psum_tensor(name, list(shape), dt))

    def _need(self, eng, reads, writes, extra=()):
        best_c, best_d = {}, {}

        def add(tok, raw):
            if tok[0] == "c":
                if tok[1] == eng and not raw:
                    return
                if self.seen[eng][tok[1]] >= tok[2]:
                    return
                if best_c.get(tok[1], -1) < tok[2]:
                    best_c[tok[1]] = tok[2]
            else:
                if self.dseen[eng][tok[1]] >= tok[2]:
                    return
                if best_d.get(tok[1], 0) < tok[2]:
                    best_d[tok[1]] = tok[2]
        for t in extra:
            add(t, True)
        for b in reads:
            for t in b.w:
                add(t, True)
        for b in writes:
            for t in b.w:
                add(t, False)
            for t in b.r:
                add(t, False)
        toks = [("c", e2, i) for e2, i in best_c.items()] + [("d", si, tg) for si, tg in best_d.items()]
        out = []
        for t in toks:
            implied = False
            if t[0] == "c":
                for u in toks:
                    if u is t:
                        continue
                    sn = self.snap[u[1]][u[2]] if u[0] == "c" else self.dsnap[(u[1], u[2])]
                    if sn.get(t[1], -1) >= t[2]:
                        implied = True
                        break
            if not implied:
                out.append(t)
        return out

    def _learn(self, eng, tok):
        if tok[0] == "c":
            sn = self.snap[tok[1]][tok[2]]
            if self.seen[eng][tok[1]] < tok[2]:
                self.seen[eng][tok[1]] = tok[2]
        else:
            sn = self.dsnap[(tok[1], tok[2])]
            if self.dseen[eng][tok[1]] < tok[2]:
                self.dseen[eng][tok[1]] = tok[2]
        for e3, i3 in sn.items():
            if self.seen[eng][e3] < i3:
                self.seen[eng][e3] = i3

    def _semval(self, tok):
        if tok[0] == "c":
            return self.sems[tok[1]][tok[2] % self.NS], tok[2] // self.NS + 1
        return self.dsems[tok[1]], tok[2]

    def _emit(self, eng, toks, fn, fold=True):
        if not fold:
            for t in toks:
                s, v = self._semval(t)
                self.engs[eng].wait_ge(s, v)
                self.nw[eng] = self.nw.get(eng, 0) + 1
                self.nwait += 1
            inst = fn(self.engs[eng])
            for t in toks:
                self._learn(eng, t)
            return inst
        for t in toks[:-1]:
            s, v = self._semval(t)
            self.engs[eng].wait_ge(s, v)
            self.nw[eng] = self.nw.get(eng, 0) + 1
            self.nwait += 1
        inst = fn(self.engs[eng])
        if toks:
            s, v = self._semval(toks[-1])
            inst.wait_op(s, v, "sem-ge")
        for t in toks:
            self._learn(eng, t)
        return inst

    def _wait(self, eng, tok, raw=True):
        if tok[0] == "c":
            if (tok[1] == eng and not raw) or self.seen[eng][tok[1]] >= tok[2]:
                return
        elif self.dseen[eng][tok[1]] >= tok[2]:
            return
        s, v = self._semval(tok)
        self.engs[eng].wait_ge(s, v)
        self.nw[eng] = self.nw.get(eng, 0) + 1
        self.nwait += 1
        self._learn(eng, tok)

    @staticmethod
    def _dom(old, new):
        return old[0] == "c" and new[0] == "c" and old[1] == new[1]

    def _mark(self, tok, reads, writes):
        for b in reads:
            b.r = [t for t in b.r if not self._dom(t, tok)]
            b.r.append(tok)
        for b in writes:
            b.w = [t for t in b.w if not self._dom(t, tok)]
            b.w.append(tok)
            b.r = []

    def op(self, eng, fn, reads=(), writes=(), fold=True):
        toks = self._need(eng, reads, writes)
        inst = self._emit(eng, toks, fn, fold=fold)
        n = self.cnt[eng]
        self.cnt[eng] = n + 1
        inst.then_inc(self.sems[eng][n % self.NS], 1)
        self.snap[eng].append(dict(self.seen[eng]))
        tok = ("c", eng, n)
        for b in writes:
            b.w = [t for t in b.w if (t[0] == "c" and t[1] == eng)]
        self._mark(tok, reads, writes)
        self.ninst += 1
        return inst

    def dma(self, eng, out, in_, reads=(), writes=(), fn=None, **kw):
        si = self.dnext
        self.dnext = (si + 1) % self.ND
        extra = [("d", si, self.dtarget[si])] if self.dtarget[si] > 0 else []
        toks = self._need(eng, reads, writes, extra=extra)
        inst = self._emit(eng, toks, fn if fn is not None else (lambda e: e.dma_start(out=out, in_=in_, **kw)))
        self.dtarget[si] += 16
        inst.then_inc(self.dsems[si], 16)
        self.dsnap[(si, self.dtarget[si])] = dict(self.seen[eng])
        self._mark(("d", si, self.dtarget[si]), reads, writes)
        self.ninst += 1
        self.nd[eng] = self.nd.get(eng, 0) + 1
        return inst

    def barrier(self, bufs=()):
        for e in self.engs:
            for e2 in self.engs:
                if e2 != e and self.cnt[e2] > 0:
                    self._wait(e, ("c", e2, self.cnt[e2] - 1))
            for si in range(self.ND):
                if self.dtarget[si] > 0:
                    self._wait(e, ("d", si, self.dtarget[si]))
        for b in bufs:
            b.w = []
            b.r = []

    def finish(self):
        for e2 in self.engs:
            if e2 != "sp" and self.cnt[e2] > 0:
                self._wait("sp", ("c", e2, self.cnt[e2] - 1))
        for si in range(self.ND):
            if self.dtarget[si] > 0:
                self._wait("sp", ("d", si, self.dtarget[si]))


SCAN_BLK = 64
SCAN_T4_ENG = "dve"
SCAN_V_ENG = "pool"
SCAN_F32R = True
SCAN_T3B_ENG = "dve"
SCAN_DIAG_NOVB = False


def scan_consts_np():
    p = np.arange(128)
    d = p // 64
    blockones = (d[:, None] == d[None, :]).astype(np.float32)
    Cy = np.zeros((128, 192), np.float32)
    Cy[p, 63 + 64 * d] = 1.0
    return {"c_blockones": blockones, "c_Cy": Cy}


def build_scan(kb, consts, scd, vrow, yd, nsteps, S, Sb, y_from=0, per_step=None, n_psa=2):
    nc = kb.nc
    blockones, Cy = consts["blockones"], consts["Cy"]
    cb = consts["buf"]
    B = SCAN_BLK
    nblk = nsteps // B
    NVB = 4
    es = ExitStack()
    sbt = lambda n, s, dt=F32: es.enter_context(nc.sbuf_tensor("sc_" + n, list(s), dt))
    pst = lambda n, s, dt=F32: es.enter_context(nc.psum_tensor("sc_" + n, list(s), dt))
    SC = [sbt(f"sc{i}", [128, 5, 16, B]) for i in range(2)]
    SCb = [Buf() for _ in range(2)]
    VB = [sbt(f"vb{i}", [128, 1024]) for i in range(NVB)]
    VBb = [Buf() for _ in range(NVB)]
    FR = mybir.dt.float32r if SCAN_F32R else F32
    T1 = sbt("t1", [128, 1024], FR); T1b = Buf()
    T2 = sbt("t2", [128, 1024]); T2b = Buf()
    T3 = [sbt(f"t3{i}", [128, 1024]) for i in range(2)]; T3b = [Buf() for _ in range(2)]
    SP = [sbt(f"sp{i}", [128, 1024]) for i in range(2)]; SPb = [Buf() for _ in range(2)]
    T4 = [sbt(f"t4{i}", [128, 1024], FR) for i in range(2)]; T4b = [Buf() for _ in range(2)]
    YS = [sbt(f"ys{i}", [128, 1024]) for i in range(2)]; YSb = [Buf() for _ in range(2)]
    S2 = sbt("s2", [128, 1024]); S2b = Buf()
    Sb_h = [[Sb, Buf()], [S2b, Buf()]]
    SSt = [S, S2]
    assert nsteps % 2 == 0
    T1hb = [Buf(), Buf()]; T2hb = [Buf(), Buf()]
    T3hb = [[Buf(), Buf()], [Buf(), Buf()]]
    pSA = [pst(f"psa{i}", [128, 1024]) for i in range(n_psa)]
    pSAhb = [[Buf(), Buf()] for _ in range(n_psa)]
    pY = pst("py", [128, 1024]); pYb = Buf()
    v3 = lambda t: t[:].rearrange("p (h i) -> p h i", i=64)
    hv = lambda t, hf: t[:, hf * 512:(hf + 1) * 512].rearrange("p (h i) -> p h i", i=64)
    T4ENG = SCAN_T4_ENG
    r32 = lambda ap: ap
    if SCAN_F32R:
        bo_r = sbt("bo_r", [128, 128], FR); cy_r = sbt("cy_r", [128, 192], FR); crb = Buf()
        kb.op("dve", lambda e: e.tensor_copy(out=bo_r[:], in_=blockones[:]), reads=[cb], writes=[crb])
        kb.op("dve", lambda e: e.tensor_copy(out=cy_r[:], in_=Cy[:]), reads=[cb], writes=[crb])
        blockones, Cy, cb = bo_r, cy_r, crb

    def load_block(bi):
        k = bi % 2
        s0 = bi * B
        for d in range(2):
            kb.dma("sp", SC[k][d * 64:(d + 1) * 64], scd[d][:, :, :, s0:s0 + B], writes=[SCb[k]])

    def issue_vb(s):
        for d in range(2):
            kb.dma("sp", VB[s % NVB][d * 64:(d + 1) * 64, :], vrow(d, s).partition_broadcast(64),
                   writes=[VBb[s % NVB]])

    def emit_y(s):
        bi, tt = divmod(s, B)
        k = bi % 2
        st = SSt[(s + 1) % 2]
        stb = Sb_h[(s + 1) % 2]
        t4, t4b = T4[s % 2], T4b[s % 2]
        r_bc = SC[k][:, 4, :, tt:tt + 1].to_broadcast([128, 16, 64])
        kb.op(T4ENG, lambda e: e.tensor_tensor(out=v3(t4), in0=v3(st), in1=r_bc, op=ALU.mult),
              reads=[stb[0], stb[1], SCb[k]], writes=[t4b])
        for hf in range(2):
            kb.op("pe", lambda e: e.matmul(pY[:, hf * 512:(hf + 1) * 512], r32(Cy[:, 63 - tt:63 - tt + 128]),
                                           t4[:, hf * 512:(hf + 1) * 512], start=(tt == 0), stop=(tt == B - 1)),
                  reads=[t4b, cb], writes=[pYb])
        if tt == B - 1:
            ys = YS[bi % 2]
            kb.op("act", lambda e: e.copy(out=ys[:], in_=pY[:]), reads=[pYb], writes=[YSb[bi % 2]])
            s0 = bi * B
            for d in range(2):
                kb.dma("sp", yd[d][s0:s0 + B, :], ys[d * 64:(d + 1) * 64, :], reads=[YSb[bi % 2]])

    load_block(0)
    for s in range(min(NVB - 1, nsteps)):
        issue_vb(s)
    for s in range(nsteps):
        bi, tt = divmod(s, B)
        k = bi % 2
        sc = lambda q: SC[k][:, q, :, tt:tt + 1].to_broadcast([128, 16, 64])
        sch = lambda q, hf: SC[k][:, q, hf * 8:(hf + 1) * 8, tt:tt + 1].to_broadcast([128, 8, 64])
        cur, curb = SSt[s % 2], Sb_h[s % 2]
        nxt, nxtb = SSt[(s + 1) % 2], Sb_h[(s + 1) % 2]
        psa, psab = pSA[s % n_psa], pSAhb[s % n_psa]
        t3, t3b = T3[s % 2], T3b[s % 2]
        sp_, spb = SP[s % 2], SPb[s % 2]
        for hf in range(2):
            kb.op("dve", lambda e: e.tensor_tensor(out=hv(T1, hf), in0=hv(cur, hf), in1=sch(1, hf), op=ALU.mult),
                  reads=[curb[hf], SCb[k]], writes=[T1hb[hf]])
            kb.op("pe", lambda e: e.matmul(psa[:, hf * 512:(hf + 1) * 512], r32(blockones[:]),
                                           T1[:, hf * 512:(hf + 1) * 512], start=True, stop=True),
                  reads=[T1hb[hf], cb], writes=[psab[hf]])
        t3h = T3hb[s % 2]
        kb.op(SCAN_T3B_ENG, lambda e: e.tensor_tensor(out=hv(t3, 1), in0=hv(VB[s % NVB], 1), in1=sch(3, 1), op=ALU.mult),
              reads=[VBb[s % NVB], SCb[k]], writes=[t3h[1]])
        if s - 1 >= y_from:
            emit_y(s - 1)
        if tt == 0 and bi + 1 < nblk:
            load_block(bi + 1)
        if s + NVB - 1 < nsteps and not SCAN_DIAG_NOVB:
            issue_vb(s + NVB - 1)
        if per_step is not None:
            per_step(s)
        kb.op("pool", lambda e: e.tensor_tensor(out=hv(t3, 0), in0=hv(VB[s % NVB], 0), in1=sch(3, 0), op=ALU.mult),
              reads=[VBb[s % NVB], SCb[k]], writes=[t3h[0]])
        kb.op("pool", lambda e: e.tensor_tensor(out=v3(sp_), in0=v3(cur), in1=sc(0), op=ALU.mult),
              reads=[curb[0], curb[1], SCb[k]], writes=[spb])
        kb.op("pool", lambda e: e.tensor_tensor(out=sp_[:], in0=sp_[:], in1=t3[:], op=ALU.add),
              reads=[spb, t3h[0], t3h[1]], writes=[spb])
        for hf in range(2):
            kb.op("dve", lambda e: e.tensor_tensor(out=hv(T2, hf), in0=hv(psa, hf), in1=sch(2, hf), op=ALU.mult),
                  reads=[psab[hf], SCb[k]], writes=[T2hb[hf]])
        for hf in range(2):
            kb.op("dve", lambda e: e.tensor_tensor(out=nxt[:, hf * 512:(hf + 1) * 512], in0=sp_[:, hf * 512:(hf + 1) * 512],
                                                   in1=T2[:, hf * 512:(hf + 1) * 512], op=ALU.subtract),
                  reads=[spb, T2hb[hf]], writes=[nxtb[hf]])
    if nsteps - 1 >= y_from:
        emit_y(nsteps - 1)
    return es


D = 1024
NX = 4096
NC_ = 256
NT = NX + NC_
EPS = 1e-6
INW = 8448
LAM_INIT = 0.2
OVERLAP_ATTN = True
FORCE_NPSA1 = False
PEER_PIPELINE = True
ATTN_LOWFP = False


class Ring:
    def __init__(self, nc, es, name, shape, dt, n, psum=False):
        alloc = nc.psum_tensor if psum else nc.sbuf_tensor
        self.t = [es.enter_context(alloc(f"{name}{i}", list(shape), dt)) for i in range(n)]
        self.b = [Buf() for _ in range(n)]
        self.i = 0

    def next(self):
        i = self.i
        self.i = (i + 1) % len(self.t)
        return self.t[i], self.b[i]


def const_inputs_np():
    c = scan_consts_np()
    c["c_ident"] = np.eye(128, dtype=np.float32)
    c["c_J"] = np.eye(128, dtype=np.float32)[::-1].copy()
    c["c_ones"] = np.ones((128, 128), np.float32)
    P = np.zeros((128, 128), np.float32)
    for dp in range(128):
        d = dp % 64
        if (d % 32) // 16 == 0:
            P[dp + 16, dp] = -1.0
        else:
            P[dp - 16, dp] = 1.0
    c["c_ropeP"] = P
    t = np.arange(NX)
    row = (t // 64).astype(np.float32)
    col = (t % 64).astype(np.float32)
    inv = (np.float32(10000.0) ** (-np.arange(16, dtype=np.float32) / np.float32(16))).astype(np.float32)
    ang = np.stack([row[:, None] * inv, col[:, None] * inv], 1).astype(np.float32)
    cos = np.ones((128, NT), np.float32)
    sin = np.zeros((128, NT), np.float32)
    for dp in range(128):
        d = dp % 64
        a, f = d // 32, d % 16
        cos[dp, NC_:] = np.cos(ang[:, a, f])
        sin[dp, NC_:] = np.sin(ang[:, a, f])
    c["c_cos"] = cos
    c["c_sin"] = sin
    cw = np.zeros((128, 255), np.float32)
    cw[:, 127] = 1.0
    c["c_Cw"] = cw
    c["c_iota"] = np.tile(np.arange(256, dtype=np.float32)[None, :], (128, 1))
    return c


INPUT_SHAPES = {
    "xT": [D, NT], "xtok": [NX, D], "cT": [D, 2],
    "w_mod": [D, 6 * D], "b_mod": [6 * D], "norm1_g": [D], "w_in": [D, INW],
    "q_norm_g": [64], "k_norm_g": [64], "diff_lambda": [4, 64], "diff_out_g": [128],
    "rw_shift": [3, 3328], "rw_w0": [2, D], "rw_w_up": [2, 64, D], "rw_a0": [2, D], "rw_a_up": [2, 64, D],
    "rw_g_up": [128, D], "rw_k_k": [D], "rw_k_a": [D], "rw_r_k": [D], "rw_ln_g": [D], "rw_ln_b": [D],
    "w_branch_a": [D, D], "w_branch_b": [D, D], "w_out": [D, D], "norm2_g": [D],
    "peer_wq": [D, 2048], "peer_keysT": [16, 128, 128], "peer_u": [16384, D], "peer_v": [16384, D],
}


class Prog:
    def __init__(self, debug=False, upto="E", dbg_keys=()):
        self.debug = debug
        self.dbg_keys = set(dbg_keys)
        self.upto = upto
        nc = bass.Bass("TRN2", target_bir_lowering=False)
        self.nc = nc
        self.kb = KB(nc)
        self.I = {k: nc.dram_tensor(k, s, F32, kind="ExternalInput").ap() for k, s in INPUT_SHAPES.items()}
        self.cnp = const_inputs_np()
        for k, v in self.cnp.items():
            self.I[k] = nc.dram_tensor(k, list(v.shape), F32, kind="ExternalInput").ap()
        self.out = nc.dram_tensor("out", [NX, D], F32, kind="ExternalOutput").ap()
        dr = lambda n, s, dt=F32: nc.dram_tensor(
            n, s, dt, kind=("ExternalOutput" if (debug and n[2:] in self.dbg_keys) else "Internal")).ap()
        self.S = {
            "QT": dr("s_QT", [8, 128, NX], BF16), "KT": dr("s_KT", [8, 128, NT], BF16),
            "VA": dr("s_VA", [NT, D], BF16), "ZR": dr("s_ZR", [26, 128, NT]), "GA": dr("s_GA", [16, 128, NX]),
            "scd0": dr("s_scd0", [64, 5, 16, NT]), "scd1": dr("s_scd1", [64, 5, 16, NT]),
            "vtok": dr("s_vtok", [NT, D]), "yd0": dr("s_yd0", [NT, D]), "yd1": dr("s_yd1", [NT, D]),
            "G1": dr("s_G1", [8, 128, NX]), "G2": dr("s_G2", [8, 128, NX]),
            "OAT": dr("s_OAT", [8, 128, NX]), "X1": dr("s_X1", [NX, D]), "H2": dr("s_H2", [NX, D], BF16), "PVB": dr("s_PVB", [16384, D], BF16),
        }
        self.Sb = {k: Buf(k) for k in self.S}
        self.build()

    def build(self):
        kb, nc, I = self.kb, self.nc, self.I
        ges = kb.es
        g = {}
        self.g = g
        cb = Buf("consts")
        g["cb"] = cb
        for nm in ["ident", "J", "ones", "ropeP", "blockones", "Cy", "Cw", "iota"]:
            shp = list(self.cnp["c_" + nm].shape)
            g[nm] = kb.sb("g_" + nm, shp)
            kb.dma("sp", g[nm][:], I["c_" + nm], writes=[cb])
        self._gt = {"g_vec": [128, 128], "g_shv": [128, 128], "g_der": [128, 32], "g_modT": [128, 16, 2],
                    "g_gs1": [128, 8, 2], "g_mrow": [128, 4, 1024], "g_lam": [128, 4]}
        self._gt = {k: kb.sb(k, v) for k, v in self._gt.items()}
        self.phase0()
        skip = getattr(self, "skip", "")
        if self.upto >= "A" and "A" not in skip:
            self.phaseA()
        if self.upto >= "B" and "B" not in skip:
            self.phaseB()
        if self.upto >= "C":
            self.phaseC()
        if self.upto >= "D":
            self.phaseD()
        if self.upto >= "E":
            self.phaseE1()
        if self.upto >= "F":
            self.phaseE2()
        kb.finish()

    def phase0(self):
        kb, nc, I, g = self.kb, self.nc, self.I, self.g
        cb = g["cb"]
        es = ExitStack()
        sbt = lambda n, s, dt=F32: es.enter_context(nc.sbuf_tensor("p0_" + n, list(s), dt))
        pst = lambda n, s, dt=F32: es.enter_context(nc.psum_tensor("p0_" + n, list(s), dt))
        st1 = sbt("st1", [128, 128]); st1b = Buf()
        st2 = sbt("st2", [128, 128]); st2b = Buf()
        kb.op("dve", lambda e: e.memset(st1[:], 0.0), writes=[st1b])
        kb.op("dve", lambda e: e.memset(st2[:], 0.0), writes=[st2b])
        rows = {}
        r = 0

        def put(name, ap, nrows):
            nonlocal r
            kb.dma("sp", st1[r:r + nrows, :], ap, writes=[st1b])
            rows[name] = r
            r += nrows
        v8 = lambda ap: ap.rearrange("(r c) -> r c", c=128)
        put("norm1_g", v8(I["norm1_g"]), 8)
        put("k_k", v8(I["rw_k_k"]), 8)
        put("k_a", v8(I["rw_k_a"]), 8)
        put("r_k", v8(I["rw_r_k"]), 8)
        put("ln_g", v8(I["rw_ln_g"]), 8)
        put("ln_b", v8(I["rw_ln_b"]), 8)
        put("w0_0", v8(I["rw_w0"][0]), 8)
        put("w0_1", v8(I["rw_w0"][1]), 8)
        put("a0_0", v8(I["rw_a0"][0]), 8)
        put("a0_1", v8(I["rw_a0"][1]), 8)
        qg2 = I["q_norm_g"].rearrange("(o c) -> o c", o=1)
        kg2 = I["k_norm_g"].rearrange("(o c) -> o c", o=1)
        rows["qg"] = r
        kb.dma("sp", st1[r:r + 1, 0:64], qg2, writes=[st1b]); kb.dma("sp", st1[r:r + 1, 64:128], qg2, writes=[st1b]); r += 1
        rows["kg"] = r
        kb.dma("sp", st1[r:r + 1, 0:64], kg2, writes=[st1b]); kb.dma("sp", st1[r:r + 1, 64:128], kg2, writes=[st1b]); r += 1
        put("b_mod", I["b_mod"][0:2048].rearrange("(r c) -> r c", c=128), 16)
        assert r <= 128
        kb.dma("sp", st2[0:78, :], I["rw_shift"].rearrange("k (c p) -> (k c) p", p=128), writes=[st2b])
        self.rows = rows
        vec = self._gt["g_vec"]; vecb = Buf()
        shv = self._gt["g_shv"]; shvb = Buf()
        pT = pst("pT", [128, 128]); pTb = Buf()
        kb.op("pe", lambda e: e.transpose(pT[:], st1[:], g["ident"][:]), reads=[st1b, cb], writes=[pTb])
        kb.op("dve", lambda e: e.tensor_copy(out=vec[:], in_=pT[:]), reads=[pTb], writes=[vecb])
        kb.op("pe", lambda e: e.transpose(pT[:], st2[:], g["ident"][:]), reads=[st2b, cb], writes=[pTb])
        kb.op("dve", lambda e: e.tensor_copy(out=shv[:], in_=pT[:]), reads=[pTb], writes=[shvb])
        g["vec"], g["vecb"], g["shv"], g["shvb"] = vec, vecb, shv, shvb
        der = self._gt["g_der"]; derb = Buf()
        g["der"], g["derb"] = der, derb
        rk = rows["k_a"]
        kb.op("dve", lambda e: e.tensor_scalar(out=der[:, 0:8], in0=vec[:, rk:rk + 8], scalar1=-1.0, scalar2=1.0,
                                               op0=ALU.mult, op1=ALU.add), reads=[vecb], writes=[derb])
        kb.op("dve", lambda e: e.tensor_scalar(out=der[:, 8:9], in0=vec[:, rows["qg"]:rows["qg"] + 1], scalar1=0.125,
                                               scalar2=None, op0=ALU.mult), reads=[vecb], writes=[derb])
        cT = sbt("cT", [128, 8, 2]); cTb = Buf()
        kb.dma("sp", cT[:], I["cT"].rearrange("(c p) m -> p c m", p=128), writes=[cTb])
        sl = sbt("sl", [128, 8, 2]); slb = Buf()
        kb.op("act", lambda e: e.activation(out=sl[:], in_=cT[:], func=AF.Silu), reads=[cTb], writes=[slb])
        slbc = sbt("slbc", [128, 8, 128]); slbcb = Buf()
        for kc in range(8):
            kb.op("dve", lambda e: e.tensor_copy(out=slbc[:, kc, :], in_=sl[:, kc, 0:1].to_broadcast([128, 128])),
                  reads=[slb], writes=[slbcb])
        wmod = I["w_mod"].rearrange("(kc p) n -> p kc n", p=128)
        modT = self._gt["g_modT"]; modTb = Buf()
        g["modT"], g["modTb"] = modT, modTb
        wr = Ring(nc, es, "p0_w", [128, 8, 512], F32, 2)
        pM = pst("pM", [128, 32]); pMb = Buf()
        for c4 in range(4):
            wt, wtb = wr.next()
            kb.dma("sp", wt[:], wmod[:, :, c4 * 512:(c4 + 1) * 512], writes=[wtb])
            for j in range(4):
                cc = c4 * 4 + j
                for kc in range(8):
                    kb.op("pe", lambda e: e.matmul(pM[:, cc * 2:cc * 2 + 2], wt[:, kc, j * 128:(j + 1) * 128], sl[:, kc, :],
                                                   start=(kc == 0), stop=(kc == 7)), reads=[wtb, slb], writes=[pMb])
        rb = rows["b_mod"]
        kb.op("dve", lambda e: e.tensor_tensor(out=modT[:], in0=pM[:].rearrange("p (c m) -> p c m", m=2),
                                               in1=vec[:, rb:rb + 16].rearrange("p (c o) -> p c o", o=1).to_broadcast([128, 16, 2]),
                                               op=ALU.add), reads=[pMb, vecb], writes=[modTb])
        gs1 = self._gt["g_gs1"]; gs1b = Buf()
        g["gs1"], g["gs1b"] = gs1, gs1b
        rn = rows["norm1_g"]
        kb.op("dve", lambda e: e.tensor_scalar(out=gs1[:], in0=modT[:, 8:16, :], scalar1=1.0, scalar2=None, op0=ALU.add),
              reads=[modTb], writes=[gs1b])
        kb.op("dve", lambda e: e.tensor_tensor(out=gs1[:], in0=gs1[:],
                                               in1=vec[:, rn:rn + 8].rearrange("p (c o) -> p c o", o=1).to_broadcast([128, 8, 2]),
                                               op=ALU.mult), reads=[gs1b, vecb], writes=[gs1b])
        mrow = self._gt["g_mrow"]; mrowb = Buf()
        g["mrow"], g["mrowb"] = mrow, mrowb
        pR = Ring(nc, es, "p0_pR", [128, 512], F32, 2, psum=True)
        bb = Ring(nc, es, "p0_bb", [128, 512], F32, 2)
        for vi, v in enumerate([2, 3, 4, 5]):
            for hf in range(2):
                c0 = v * 1024 + hf * 512
                wt, wtb = wr.next()
                kb.dma("sp", wt[:], wmod[:, :, c0:c0 + 512], writes=[wtb])
                bt, btb = bb.next()
                kb.dma("sp", bt[:], I["b_mod"][c0:c0 + 512].partition_broadcast(128), writes=[btb])
                pr, prb = pR.next()
                for kc in range(8):
                    kb.op("pe", lambda e: e.matmul(pr[:], slbc[:, kc, :], wt[:, kc, :], start=(kc == 0), stop=(kc == 7)),
                          reads=[wtb, slbcb], writes=[prb])
                kb.op("dve", lambda e: e.tensor_tensor(out=mrow[:, vi, hf * 512:(hf + 1) * 512], in0=pr[:], in1=bt[:], op=ALU.add),
                      reads=[prb, btb], writes=[mrowb])
        n2 = sbt("n2", [128, 1024]); n2b = Buf()
        kb.dma("sp", n2[:], I["norm2_g"].partition_broadcast(128), writes=[n2b])
        kb.op("dve", lambda e: e.scalar_tensor_tensor(out=mrow[:, 2, :], in0=mrow[:, 2, :], scalar=1.0, in1=n2[:],
                                                      op0=ALU.add, op1=ALU.mult), reads=[mrowb, n2b], writes=[mrowb])
        dl = sbt("dl", [128, 256]); dlb = Buf()
        kb.dma("sp", dl[:], I["diff_lambda"].rearrange("a b -> (a b)").partition_broadcast(128), writes=[dlb])
        lam = self._gt["g_lam"]; lamb = Buf()
        g["lam"], g["lamb"] = lam, lamb
        pr_ = sbt("lpr", [128, 128]); prb_ = Buf()
        kb.op("dve", lambda e: e.tensor_tensor(out=pr_[:].rearrange("p (a c) -> p a c", a=2),
                                               in0=dl[:].rearrange("p (a t c) -> p a t c", a=2, t=2)[:, :, 0, :],
                                               in1=dl[:].rearrange("p (a t c) -> p a t c", a=2, t=2)[:, :, 1, :], op=ALU.mult),
              reads=[dlb], writes=[prb_])
        kb.op("dve", lambda e: e.tensor_reduce(out=lam[:, 0:2], in_=pr_[:].rearrange("p (a c) -> p a c", a=2), axis=AX.X, op=ALU.add),
              reads=[prb_], writes=[lamb])
        kb.op("act", lambda e: e.activation(out=lam[:, 0:2], in_=lam[:, 0:2], func=AF.Exp), reads=[lamb], writes=[lamb])
        kb.op("dve", lambda e: e.tensor_tensor(out=lam[:, 2:3], in0=lam[:, 0:1], in1=lam[:, 1:2], op=ALU.subtract),
              reads=[lamb], writes=[lamb])
        kb.op("dve", lambda e: e.tensor_scalar(out=lam[:, 3:4], in0=lam[:, 2:3], scalar1=LAM_INIT, scalar2=-1.0,
                                               op0=ALU.add, op1=ALU.mult), reads=[lamb], writes=[lamb])
        kb.barrier(list(self.Sb.values()))
        es.close()

    def phaseA(self):
        kb, nc, I, g, S, Sb = self.kb, self.nc, self.I, self.g, self.S, self.Sb
        cb, vec, vecb, der, derb, rows = g["cb"], g["vec"], g["vecb"], g["der"], g["derb"], self.rows
        gs1, gs1b, modT, modTb = g["gs1"], g["gs1b"], g["modT"], g["modTb"]
        es = ExitStack()
        sbt = lambda n, s, dt=F32: es.enter_context(nc.sbuf_tensor("pa_" + n, list(s), dt))
        pst = lambda n, s, dt=F32: es.enter_context(nc.psum_tensor("pa_" + n, list(s), dt))
        xTv = I["xT"].rearrange("(c p) t -> p c t", p=128)
        winv = I["w_in"].rearrange("(kc p) n -> p kc n", p=128)
        xr = Ring(nc, es, "pa_x", [128, 8, 512], F32, 2)
        sq = sbt("sq", [128, 8, 512]); sqb = Buf()
        hT = sbt("hT", [128, 8, 512], BF16); hTb = Buf()
        std = sbt("std", [128, 512]); stdb = Buf()
        rstd = sbt("rstd", [128, 512]); rstdb = Buf()
        cs = sbt("cs", [128, 2, 512]); csb = Buf()
        wk = Ring(nc, es, "pa_wk", [128, 512], F32, 12)
        wkb = Ring(nc, es, "pa_wkb", [128, 512], BF16, 4)
        wr = Ring(nc, es, "pa_w", [128, 8, 128], BF16, 4)
        wvr = Ring(nc, es, "pa_wv", [128, 8, 512], BF16, 2)
        pZ = Ring(nc, es, "pa_pZ", [128, 512], F32, 3, psum=True)
        pS = Ring(nc, es, "pa_pS", [128, 512], F32, 2, psum=True)
        pRr = Ring(nc, es, "pa_pR", [128, 512], F32, 2, psum=True)
        pN = pst("pN", [128, 512]); pNb = Buf()
        groups = [(0, 256, 1)] + [(256 + 512 * i, 512, 0) for i in range(8)]
        for (t0, n, m) in groups:
            xt, xtb = xr.next()
            kb.dma("sp", xt[:, :, :n], xTv[:, :, t0:t0 + n], writes=[xtb])
            kb.dma("sp", cs[:, 0, :n], I["c_cos"][:, t0:t0 + n], writes=[csb])
            kb.dma("sp", cs[:, 1, :n], I["c_sin"][:, t0:t0 + n], writes=[csb])
            kb.op("act", lambda e: e.activation(out=sq[:, :, :n], in_=xt[:, :, :n], func=AF.Square), reads=[xtb], writes=[sqb])
            for c in range(8):
                kb.op("pe", lambda e: e.matmul(pN[:, :n], g["ones"][:], sq[:, c, :n], start=(c == 0), stop=(c == 7)),
                      reads=[sqb, cb], writes=[pNb])
            kb.op("act", lambda e: e.activation(out=std[:, :n], in_=pN[:, :n], func=AF.Sqrt, bias=EPS, scale=1.0 / D),
                  reads=[pNb], writes=[stdb])
            kb.op("dve", lambda e: e.reciprocal(out=rstd[:, :n], in_=std[:, :n]), reads=[stdb], writes=[rstdb])
            for c in range(8):
                tm, tmb = wk.next()
                kb.op("dve", lambda e: e.tensor_tensor(out=tm[:, :n], in0=xt[:, c, :n], in1=rstd[:, :n], op=ALU.mult),
                      reads=[xtb, rstdb], writes=[tmb])
                kb.op("act", lambda e: e.activation(out=hT[:, c, :n], in_=tm[:, :n], func=AF.Identity,
                                                    scale=gs1[:, c, m:m + 1], bias=modT[:, c, m:m + 1]),
                      reads=[tmb, gs1b, modTb], writes=[hTb])

            def proj_fm(col0):
                wt, wtb = wr.next()
                kb.dma("pool", wt[:], winv[:, :, col0:col0 + 128], writes=[wtb])
                pz, pzb = pZ.next()
                for kc in range(8):
                    kb.op("pe", lambda e: e.matmul(pz[:, :n], wt[:, kc, :], hT[:, kc, :n], start=(kc == 0), stop=(kc == 7)),
                          reads=[wtb, hTb], writes=[pzb])
                return pz, pzb

            for cc in range(16):
                isq = cc < 8
                if isq and m == 1:
                    continue
                pz, pzb = proj_fm(cc * 128)
                gcol = der[:, 8:9] if isq else vec[:, rows["kg"]:rows["kg"] + 1]
                sqv, sqvb = wk.next()
                kb.op("act", lambda e: e.activation(out=sqv[:, :n], in_=pz[:, :n], func=AF.Square), reads=[pzb], writes=[sqvb])
                qg, qgb = wk.next()
                kb.op("act", lambda e: e.activation(out=qg[:, :n], in_=pz[:, :n], func=AF.Identity, scale=gcol),
                      reads=[pzb, derb, vecb], writes=[qgb])
                ps_, psb = pS.next()
                kb.op("pe", lambda e: e.matmul(ps_[:, :n], g["blockones"][:], sqv[:, :n], start=True, stop=True),
                      reads=[sqvb, cb], writes=[psb])
                pr_, prb = pRr.next()
                kb.op("pe", lambda e: e.matmul(pr_[:, :n], g["ropeP"][:], qg[:, :n], start=True, stop=True),
                      reads=[qgb, cb], writes=[prb])
                sd, sdb = wk.next()
                kb.op("act", lambda e: e.activation(out=sd[:, :n], in_=ps_[:, :n], func=AF.Sqrt, bias=EPS, scale=1.0 / 64),
                      reads=[psb], writes=[sdb])
                rs, rsb = wk.next()
                kb.op("dve", lambda e: e.reciprocal(out=rs[:, :n], in_=sd[:, :n]), reads=[sdb], writes=[rsb])
                t1, t1b = wk.next()
                kb.op("dve", lambda e: e.tensor_tensor(out=t1[:, :n], in0=qg[:, :n], in1=cs[:, 0, :n], op=ALU.mult),
                      reads=[qgb, csb], writes=[t1b])
                t2, t2b = wk.next()
                kb.op("dve", lambda e: e.tensor_tensor(out=t2[:, :n], in0=pr_[:, :n], in1=cs[:, 1, :n], op=ALU.mult),
                      reads=[prb, csb], writes=[t2b])
                kb.op("dve", lambda e: e.tensor_tensor(out=t1[:, :n], in0=t1[:, :n], in1=t2[:, :n], op=ALU.add),
                      reads=[t1b, t2b], writes=[t1b])
                qf, qfb = wkb.next()
                kb.op("dve", lambda e: e.tensor_tensor(out=qf[:, :n], in0=t1[:, :n], in1=rs[:, :n], op=ALU.mult),
                      reads=[t1b, rsb], writes=[qfb])
                if isq:
                    kb.dma("sp", S["QT"][cc, :, t0 - NC_:t0 - NC_ + n], qf[:, :n], reads=[qfb], writes=[Sb["QT"]])
                else:
                    kb.dma("sp", S["KT"][cc - 8, :, t0:t0 + n], qf[:, :n], reads=[qfb], writes=[Sb["KT"]])
            for hf in range(2):
                wv, wvb = wvr.next()
                kb.dma("pool", wv[:], winv[:, :, 2048 + hf * 512:2048 + (hf + 1) * 512], writes=[wvb])
                for ti in range(n // 128):
                    pz, pzb = pZ.next()
                    for kc in range(8):
                        kb.op("pe", lambda e: e.matmul(pz[:], hT[:, kc, ti * 128:(ti + 1) * 128], wv[:, kc, :],
                                                       start=(kc == 0), stop=(kc == 7)), reads=[wvb, hTb], writes=[pzb])
                    ob, obb = wkb.next()
                    kb.op("act", lambda e: e.copy(out=ob[:], in_=pz[:]), reads=[pzb], writes=[obb])
                    kb.dma("sp", S["VA"][t0 + ti * 128:t0 + (ti + 1) * 128, hf * 512:(hf + 1) * 512], ob[:],
                           reads=[obb], writes=[Sb["VA"]])
            for c in range(26):
                pz, pzb = proj_fm(3072 + c * 128)
                ob, obb = wk.next()
                kb.op("act", lambda e: e.copy(out=ob[:, :n], in_=pz[:, :n]), reads=[pzb], writes=[obb])
                kb.dma("sp", S["ZR"][c, :, t0:t0 + n], ob[:, :n], reads=[obb], writes=[Sb["ZR"]])
            if m == 0:
                for c in range(16):
                    pz, pzb = proj_fm(6400 + c * 128)
                    ob, obb = wk.next()
                    kb.op("act", lambda e: e.activation(out=ob[:, :n], in_=pz[:, :n], func=AF.Sigmoid), reads=[pzb], writes=[obb])
                    kb.dma("sp", S["GA"][c, :, t0 - NC_:t0 - NC_ + n], ob[:, :n], reads=[obb], writes=[Sb["GA"]])
        kb.barrier(list(self.Sb.values()))
        es.close()

    def phaseB(self):
        kb, nc, I, g, S, Sb = self.kb, self.nc, self.I, self.g, self.S, self.Sb
        cb, vec, vecb, der, derb, rows = g["cb"], g["vec"], g["vecb"], g["der"], g["derb"], self.rows
        shv, shvb = g["shv"], g["shvb"]
        es = ExitStack()
        sbt = lambda n, s, dt=F32: es.enter_context(nc.sbuf_tensor("pb_" + n, list(s), dt))
        n = 256
        zs = sbt("zs", [128, 26, n]); zsb = Buf()
        zring = Ring(nc, es, "pb_zr", [128, n + 2], F32, 3)
        tw = sbt("tw", [128, n]); twb = Buf()
        sg = sbt("sg", [128, n]); sgb = Buf()
        wk = Ring(nc, es, "pb_wk", [128, n], F32, 28)
        stgr = Ring(nc, es, "pb_stg", [128, 5, n], F32, 4)
        vtr = Ring(nc, es, "pb_vt", [128, 1024], F32, 2)
        pr = Ring(nc, es, "pb_p", [128, 512], F32, 4, psum=True)
        pbig = es.enter_context(nc.psum_tensor("pb_big", [128, 1024], F32)); pbigb = Buf()
        wup = sbt("wup", [64, 2, 1024]); wupb = Buf()
        aup = sbt("aup", [128, 2, 1024]); aupb = Buf()
        gup = sbt("gup", [128, 1024]); gupb = Buf()
        for d in range(2):
            kb.dma("sp", wup[:, d, :], I["rw_w_up"][d], writes=[wupb])
            kb.dma("sp", aup[64:128, d, :], I["rw_a_up"][d], writes=[aupb])
        kb.dma("sp", gup[:], I["rw_g_up"], writes=[gupb])
        col = lambda name, c: vec[:, rows[name] + c:rows[name] + c + 1]
        DEC = -float(np.exp(-0.5))
        groups = [(0, 0, NC_)] + [(NC_ + n * i, NC_, NT) for i in range(NX // n)]
        for (t0, s_lo, s_hi) in groups:
            isctx = t0 < NC_
            for c in range(26):
                zr, zrb = zring.next()
                lo = t0 - 1 if t0 > s_lo else t0
                hi = t0 + n + 1 if t0 + n < s_hi else t0 + n
                if t0 == s_lo:
                    kb.op("dve", lambda e: e.memset(zr[:, 0:1], 0.0), writes=[zrb])
                if t0 + n == s_hi:
                    kb.op("dve", lambda e: e.memset(zr[:, n + 1:n + 2], 0.0), writes=[zrb])
                kb.dma("sp", zr[:, lo - (t0 - 1):hi - (t0 - 1)], S["ZR"][c, :, lo:hi], reads=[Sb["ZR"]], writes=[zrb])
                w0_, w1_, w2_ = (shv[:, k * 26 + c:k * 26 + c + 1] for k in range(3))
                kb.op("dve", lambda e: e.tensor_scalar(out=zs[:, c, :], in0=zr[:, 1:n + 1], scalar1=w1_, scalar2=None, op0=ALU.mult),
                      reads=[zrb, shvb], writes=[zsb])
                kb.op("dve", lambda e: e.scalar_tensor_tensor(out=zs[:, c, :], in0=zr[:, 0:n], scalar=w0_, in1=zs[:, c, :],
                                                              op0=ALU.mult, op1=ALU.add), reads=[zrb, shvb, zsb], writes=[zsb])
                kb.op("dve", lambda e: e.scalar_tensor_tensor(out=zs[:, c, :], in0=zr[:, 2:n + 2], scalar=w2_, in1=zs[:, c, :],
                                                              op0=ALU.mult, op1=ALU.add), reads=[zrb, shvb, zsb], writes=[zsb])
            kb.op("act", lambda e: e.activation(out=tw[0:64, :], in_=zs[0:64, 24, :], func=AF.Tanh), reads=[zsb], writes=[twb])
            if not isctx:
                kb.op("act", lambda e: e.activation(out=sg[:], in_=zs[:, 25, :], func=AF.Sigmoid), reads=[zsb], writes=[sgb])
            s0 = [t0, 0 if isctx else NT - t0]
            for c in range(8):
                A = []
                STG = [stgr.next(), stgr.next()]
                for d in range(2):
                    pw, pwb = pr.next()
                    kb.op("pe", lambda e: e.matmul(pw[:, :n], wup[0:64, d, c * 128:(c + 1) * 128], tw[0:64, :], start=True, stop=True),
                          reads=[wupb, twb], writes=[pwb])
                    s1, s1b = wk.next()
                    kb.op("act", lambda e: e.activation(out=s1[:], in_=pw[:, :n], func=AF.Sigmoid, bias=col("w0_%d" % d, c)),
                          reads=[pwb, vecb], writes=[s1b])
                    stg, stgb = STG[d]
                    kb.op("act", lambda e: e.activation(out=(stg[:, 0, ::-1] if d else stg[:, 0, :]), in_=s1[:], func=AF.Exp, scale=DEC),
                          reads=[s1b], writes=[stgb])
                    pa, pab = pr.next()
                    kb.op("pe", lambda e: e.matmul(pa[:, :n], aup[64:128, d, c * 128:(c + 1) * 128], zs[64:128, 24, :], start=True, stop=True),
                          reads=[aupb, zsb], writes=[pab])
                    ad, adb = wk.next()
                    kb.op("act", lambda e: e.activation(out=ad[:], in_=pa[:, :n], func=AF.Sigmoid, bias=col("a0_%d" % d, c)),
                          reads=[pab, vecb], writes=[adb])
                    A.append((ad, adb))
                kq, kqb = wk.next()
                kb.op("act", lambda e: e.activation(out=kq[:], in_=zs[:, 8 + c, :], func=AF.Identity, scale=col("k_k", c)),
                      reads=[zsb, vecb], writes=[kqb])
                sqk, sqkb = wk.next()
                kb.op("act", lambda e: e.activation(out=sqk[:], in_=kq[:], func=AF.Square), reads=[kqb], writes=[sqkb])
                pk, pkb = pr.next()
                kb.op("pe", lambda e: e.matmul(pk[:, :n], g["blockones"][:], sqk[:], start=True, stop=True), reads=[sqkb, cb], writes=[pkb])
                sd, sdb = wk.next()
                kb.op("act", lambda e: e.activation(out=sd[:], in_=pk[:, :n], func=AF.Sqrt, bias=1e-12), reads=[pkb], writes=[sdb])
                rn, rnb = wk.next()
                kb.op("dve", lambda e: e.reciprocal(out=rn[:], in_=sd[:]), reads=[sdb], writes=[rnb])
                kk, kkb = wk.next()
                kb.op("dve", lambda e: e.tensor_tensor(out=kk[:], in0=kq[:], in1=rn[:], op=ALU.mult), reads=[kqb, rnb], writes=[kkb])
                kb.op("pool", lambda e: e.tensor_copy(out=STG[0][0][:, 1, :], in_=kk[:]), reads=[kkb], writes=[STG[0][1]])
                kb.op("pool", lambda e: e.tensor_copy(out=STG[1][0][:, 1, ::-1], in_=kk[:]), reads=[kkb], writes=[STG[1][1]])
                for d in range(2):
                    ad, adb = A[d]
                    tm, tmb = wk.next()
                    kb.op("dve", lambda e: e.tensor_scalar(out=tm[:], in0=ad[:], scalar1=col("k_a", c), scalar2=der[:, c:c + 1],
                                                           op0=ALU.mult, op1=ALU.add), reads=[adb, vecb, derb], writes=[tmb])
                    stg, stgb = STG[d]
                    kb.op("dve", lambda e: e.tensor_tensor(out=(stg[:, 3, ::-1] if d else stg[:, 3, :]), in0=tm[:], in1=zs[:, 8 + c, :], op=ALU.mult),
                          reads=[tmb, zsb], writes=[stgb])
                    kb.op("dve", lambda e: e.tensor_tensor(out=(stg[:, 2, ::-1] if d else stg[:, 2, :]), in0=kk[:], in1=ad[:], op=ALU.mult),
                          reads=[kkb, adb], writes=[stgb])
                kb.op("pool", lambda e: e.tensor_copy(out=STG[0][0][:, 4, :], in_=zs[:, c, :]), reads=[zsb], writes=[STG[0][1]])
                kb.op("pool", lambda e: e.tensor_copy(out=STG[1][0][:, 4, ::-1], in_=zs[:, c, :]), reads=[zsb], writes=[STG[1][1]])
                for d in range(2):
                    stg, stgb = STG[d]
                    dst = S["scd%d" % d]
                    for b in range(2):
                        kb.dma("pool" if (d + b) % 2 else "act", dst[:, :, 2 * c + b, s0[d]:s0[d] + n], stg[b * 64:(b + 1) * 64, :, :],
                               reads=[stgb], writes=[Sb["scd%d" % d]])
                if not isctx:
                    rk, rkb = wk.next()
                    kb.op("dve", lambda e: e.scalar_tensor_tensor(out=rk[:], in0=zs[:, c, :], scalar=col("r_k", c), in1=zs[:, 8 + c, :],
                                                                  op0=ALU.mult, op1=ALU.mult), reads=[zsb, vecb], writes=[rkb])
                    pb_, pbb = pr.next()
                    kb.op("pe", lambda e: e.matmul(pb_[:, :n], g["blockones"][:], rk[:], start=True, stop=True), reads=[rkb, cb], writes=[pbb])
                    bon, bonb = wk.next()
                    kb.op("dve", lambda e: e.tensor_tensor(out=bon[:], in0=pb_[:, :n], in1=zs[:, 16 + c, :], op=ALU.mult),
                          reads=[pbb, zsb], writes=[bonb])
                    pg, pgb = pr.next()
                    kb.op("pe", lambda e: e.matmul(pg[:, :n], gup[:, c * 128:(c + 1) * 128], sg[:], start=True, stop=True),
                          reads=[gupb, sgb], writes=[pgb])
                    g1, g1b = wk.next()
                    kb.op("dve", lambda e: e.tensor_scalar(out=g1[:], in0=pg[:, :n], scalar1=col("ln_g", c), scalar2=None, op0=ALU.mult),
                          reads=[pgb, vecb], writes=[g1b])
                    g2, g2b = wk.next()
                    kb.op("dve", lambda e: e.scalar_tensor_tensor(out=g2[:], in0=bon[:], scalar=col("ln_b", c), in1=pg[:, :n],
                                                                  op0=ALU.add, op1=ALU.mult), reads=[bonb, pgb, vecb], writes=[g2b])
                    kb.dma("act", S["G1"][c, :, t0 - NC_:t0 - NC_ + n], g1[:], reads=[g1b], writes=[Sb["G1"]])
                    kb.dma("pool", S["G2"][c, :, t0 - NC_:t0 - NC_ + n], g2[:], reads=[g2b], writes=[Sb["G2"]])
            for hf in range(n // 128):
                for c in range(8):
                    kb.op("pe", lambda e: e.transpose(pbig[:, c * 128:(c + 1) * 128], zs[:, 16 + c, hf * 128:(hf + 1) * 128], g["ident"][:]),
                          reads=[zsb, cb], writes=[pbigb])
                vt, vtb = vtr.next()
                kb.op("act", lambda e: e.copy(out=vt[:], in_=pbig[:]), reads=[pbigb], writes=[vtb])
                kb.dma("pool", S["vtok"][t0 + hf * 128:t0 + (hf + 1) * 128, :], vt[:], reads=[vtb], writes=[Sb["vtok"]])
        kb.barrier(list(self.Sb.values()))
        es.close()

    def phaseC(self):
        kb, nc, I, g, S, Sb = self.kb, self.nc, self.I, self.g, self.S, self.Sb
        es = ExitStack()
        St = es.enter_context(nc.sbuf_tensor("pc_S", [128, 1024], F32)); Stb = Buf()
        kb.op("dve", lambda e: e.memset(St[:], 0.0), writes=[Stb])
        consts = {"blockones": g["blockones"], "Cy": g["Cy"], "buf": g["cb"]}

        def vrow(d, s):
            if d == 0:
                t = s
            else:
                t = (NC_ - 1 - s) if s < NC_ else (NT + NC_ - 1 - s)
            return S["vtok"][t, :]
        nsteps = getattr(self, "scan_steps", NT)
        gen = None
        if OVERLAP_ATTN and self.upto >= "D":
            gen = self.attn_gen(es, overlap=True)
            self.overlapped = True
            next(gen, None)
        pvg = None
        if gen is not None and self.upto >= "F":
            pvg = self.pvb_gen(es)
            next(pvg, None)

        def per_step(s):
            if gen is not None:
                next(gen, None)
            if pvg is not None and s % 32 == 0:
                next(pvg, None)
        es2 = build_scan(kb, consts, [S["scd0"], S["scd1"]], vrow, [S["yd0"], S["yd1"]], nsteps, St, Stb, y_from=NC_,
                         per_step=per_step, n_psa=(1 if (gen is not None or FORCE_NPSA1) else 2))
        if gen is not None:
            for _ in gen:
                pass
        if pvg is not None:
            for _ in pvg:
                pass
        if self.debug:
            sd = nc.dram_tensor("dbg_S", [128, 1024], F32, kind="ExternalOutput").ap()
            kb.dma("sp", sd, St[:], reads=[Stb])
        kb.barrier(list(self.Sb.values()))
        es2.close()
        es.close()

    def attn_gen(self, es, overlap):
        kb, nc, I, g, S, Sb = self.kb, self.nc, self.I, self.g, self.S, self.Sb
        cb, lam, lamb = g["cb"], g["lam"], g["lamb"]
        sbt = lambda n, s, dt=F32: es.enter_context(nc.sbuf_tensor("pd_" + n, list(s), dt))
        NKT = NT // 128
        nb = 1 if overlap else 2
        KTr = Ring(nc, es, "pd_K", [128, NT], BF16, nb)
        QTr = Ring(nc, es, "pd_Q", [128, NX], BF16, nb)
        Vr = Ring(nc, es, "pd_V", [128, NKT, 128], BF16, nb)
        onesb = sbt("onesb", [128, 128], BF16); onesbb = Buf()
        kb.op("dve", lambda e: e.memset(onesb[:], 1.0), writes=[onesbb])
        og = sbt("og", [128, 1]); ogb = Buf()
        kb.dma("sp", og[:], I["diff_out_g"].rearrange("(p o) -> p o", o=1), writes=[ogb])
        kb.op("dve", lambda e: e.tensor_scalar(out=og[:], in0=og[:], scalar1=1.0 - LAM_INIT, scalar2=None, op0=ALU.mult),
              reads=[ogb], writes=[ogb])
        PTr = Ring(nc, es, "pd_PT", [128, 512], BF16, 4)
        wk = Ring(nc, es, "pd_wk", [128, 512], F32, 7)
        pS = Ring(nc, es, "pd_pS", [128, 512], F32, 2 if overlap else 3, psum=True)
        pO = es.enter_context(nc.psum_tensor("pd_pO", [128, 512], F32)); pOb = Buf()
        pZ = es.enter_context(nc.psum_tensor("pd_pZ", [128, 512], F32)); pZb = Buf()
        for h in range(8):
            kt_, ktb = KTr.next(); qt_, qtb = QTr.next(); vh, vhb = Vr.next()
            kb.dma("sp", kt_[:], S["KT"][h], reads=[Sb["KT"]], writes=[ktb])
            kb.dma("act", qt_[:], S["QT"][h], reads=[Sb["QT"]], writes=[qtb])
            vav = S["VA"][:, h * 128:(h + 1) * 128].rearrange("(kt p) c -> p kt c", p=128)
            for k2 in range(0, NKT, 2):
                kb.dma("sp" if (k2 // 2) % 2 else "act", vh[:, k2:k2 + 2, :], vav[:, k2:k2 + 2, :], reads=[Sb["VA"]], writes=[vhb])
            for qg in range(NX // 512):
                units = [(m, kt) for m in range(2) for kt in range(NKT)]

                def issue_scores(u):
                    m, kt = u
                    ps, psb = pS.next()
                    kb.op("pe", lambda e: e.matmul(ps[:], kt_[m * 64:(m + 1) * 64, kt * 128:(kt + 1) * 128],
                                                   qt_[m * 64:(m + 1) * 64, qg * 512:(qg + 1) * 512], start=True, stop=True),
                          reads=[ktb, qtb], writes=[psb])
                    return ps, psb
                nxt_s = issue_scores(units[0])
                maps = []
                for i, (m, kt) in enumerate(units):
                    ps, psb = nxt_s
                    if i + 1 < len(units):
                        nxt_s = issue_scores(units[i + 1])
                    pt, ptb = PTr.next()
                    kb.op("act", lambda e: e.activation(out=pt[:], in_=ps[:], func=AF.Exp), reads=[psb], writes=[ptb])
                    kb.op("pe", lambda e: e.matmul(pO[:], vh[:, kt, :], pt[:], start=(kt == 0), stop=(kt == NKT - 1)),
                          reads=[vhb, ptb], writes=[pOb])
                    kb.op("pe", lambda e: e.matmul(pZ[:], onesb[:], pt[:], start=(kt == 0), stop=(kt == NKT - 1)),
                          reads=[onesbb, ptb], writes=[pZb])
                    if kt == NKT - 1:
                        r_, rb_ = wk.next()
                        kb.op("dve", lambda e: e.reciprocal(out=r_[:], in_=pZ[:]), reads=[pZb], writes=[rb_])
                        a_, ab_ = wk.next()
                        kb.op("dve", lambda e: e.tensor_tensor(out=a_[:], in0=pO[:], in1=r_[:], op=ALU.mult), reads=[pOb, rb_], writes=[ab_])
                        maps.append((a_, ab_))
                    yield
                (a_, ab_), (b_, bb_) = maps
                kb.op("dve", lambda e: e.scalar_tensor_tensor(out=a_[:], in0=b_[:], scalar=lam[:, 3:4], in1=a_[:], op0=ALU.mult, op1=ALU.add),
                      reads=[ab_, bb_, lamb], writes=[ab_])
                sq, sqb = wk.next()
                kb.op("act", lambda e: e.activation(out=sq[:], in_=a_[:], func=AF.Square), reads=[ab_], writes=[sqb])
                pn, pnb = pS.next()
                kb.op("pe", lambda e: e.matmul(pn[:], g["ones"][:], sq[:], start=True, stop=True), reads=[sqb, cb], writes=[pnb])
                sd, sdb = wk.next()
                kb.op("act", lambda e: e.activation(out=sd[:], in_=pn[:], func=AF.Sqrt, bias=EPS, scale=1.0 / 128), reads=[pnb], writes=[sdb])
                kb.op("dve", lambda e: e.reciprocal(out=sd[:], in_=sd[:]), reads=[sdb], writes=[sdb])
                kb.op("dve", lambda e: e.tensor_tensor(out=a_[:], in0=a_[:], in1=sd[:], op=ALU.mult), reads=[ab_, sdb], writes=[ab_])
                o_, ob_ = wk.next()
                kb.op("act", lambda e: e.activation(out=o_[:], in_=a_[:], func=AF.Identity, scale=og[:, 0:1]), reads=[ab_, ogb], writes=[ob_])
                kb.dma("pool", S["OAT"][h, :, qg * 512:(qg + 1) * 512], o_[:], reads=[ob_], writes=[Sb["OAT"]])

    def pvb_gen(self, es):
        kb, nc, I, S, Sb = self.kb, self.nc, self.I, self.S, self.Sb
        fr = Ring(nc, es, "pv_f", [128, 1024], F32, 2)
        br = Ring(nc, es, "pv_b", [128, 1024], BF16, 2)
        yield
        for i in range(128):
            tf, tfb = fr.next()
            kb.dma("sp", tf[:], I["peer_v"][i * 128:(i + 1) * 128, :], writes=[tfb])
            tb, tbb = br.next()
            kb.op("act", lambda e: e.copy(out=tb[:], in_=tf[:]), reads=[tfb], writes=[tbb])
            kb.dma("act", S["PVB"][i * 128:(i + 1) * 128, :], tb[:], reads=[tbb], writes=[Sb["PVB"]])
            yield
        self.pvb_done = True

    def phaseD(self):
        if getattr(self, "overlapped", False):
            return
        kb = self.kb
        es = ExitStack()
        for _ in self.attn_gen(es, overlap=ATTN_LOWFP):
            pass
        kb.barrier(list(self.Sb.values()))
        es.close()

    def phaseE1(self):
        kb, nc, I, g, S, Sb = self.kb, self.nc, self.I, self.g, self.S, self.Sb
        cb, mrow, mrowb = g["cb"], g["mrow"], g["mrowb"]
        es = ExitStack()
        sbt = lambda n, s, dt=F32: es.enter_context(nc.sbuf_tensor("e1_" + n, list(s), dt))
        yr0 = Ring(nc, es, "e1_y0", [128, 1024], F32, 2)
        yr1 = Ring(nc, es, "e1_y1", [128, 1024], F32, 2)
        obT = sbt("obT", [128, 8, 512]); obTb = Buf()
        oaT = sbt("oaT", [128, 8, 512], BF16); oaTb = Buf()
        obH = sbt("obH", [128, 8, 512], BF16); obHb = Buf()
        mg = sbt("mg", [128, 8, 512], BF16); mgb = Buf()
        wr = Ring(nc, es, "e1_w", [128, 8, 128], BF16, 4)
        wo = Ring(nc, es, "e1_wo", [128, 8, 512], BF16, 2)
        gr = Ring(nc, es, "e1_g", [128, 512], F32, 6)
        wk = Ring(nc, es, "e1_wk", [128, 512], F32, 8)
        xr = Ring(nc, es, "e1_x", [128, 1024], F32, 2)
        x1r = Ring(nc, es, "e1_x1", [128, 1024], F32, 2)
        pT = Ring(nc, es, "e1_p", [128, 512], F32, 4, psum=True)
        pX = es.enter_context(nc.psum_tensor("e1_pX", [128, 1024], F32)); pXb = Buf()
        wav = I["w_branch_a"].rearrange("(kc p) n -> p kc n", p=128)
        wbv = I["w_branch_b"].rearrange("(kc p) n -> p kc n", p=128)
        wov = I["w_out"].rearrange("(kc p) n -> p kc n", p=128)
        for gi in range(NX // 512):
            t0 = gi * 512
            for ti in range(4):
                tt0 = t0 + ti * 128
                y0, y0b = yr0.next(); y1, y1b = yr1.next()
                kb.dma("sp", y0[:], S["yd0"][NC_ + tt0:NC_ + tt0 + 128, :], reads=[Sb["yd0"]], writes=[y0b])
                r0 = NT - 128 - tt0
                kb.dma("act", y1[:], S["yd1"][r0:r0 + 128, :], reads=[Sb["yd1"]], writes=[y1b])
                for c4 in range(2):
                    py, pyb = pT.next()
                    for cc in range(4):
                        c = c4 * 4 + cc
                        kb.op("pe", lambda e: e.matmul(py[:, cc * 128:(cc + 1) * 128], y0[:, c * 128:(c + 1) * 128], g["ident"][:],
                                                       start=True, stop=False), reads=[y0b, cb], writes=[pyb])
                        kb.op("pe", lambda e: e.matmul(py[:, cc * 128:(cc + 1) * 128], y1[:, c * 128:(c + 1) * 128], g["J"][:],
                                                       start=False, stop=True), reads=[y1b, cb], writes=[pyb])
                    kb.op("act", lambda e: e.copy(out=obT[:, c4 * 4:(c4 + 1) * 4, ti * 128:(ti + 1) * 128],
                                                  in_=py[:].rearrange("p (c t) -> p c t", t=128)), reads=[pyb], writes=[obTb])
            for c in range(8):
                pm, pmb = pT.next()
                kb.op("pe", lambda e: e.matmul(pm[:], g["blockones"][:], obT[:, c, :], start=True, stop=True), reads=[obTb, cb], writes=[pmb])
                yc, ycb = wk.next()
                kb.op("dve", lambda e: e.scalar_tensor_tensor(out=yc[:], in0=pm[:], scalar=-1.0 / 64, in1=obT[:, c, :], op0=ALU.mult, op1=ALU.add),
                      reads=[pmb, obTb], writes=[ycb])
                sq, sqb = wk.next()
                kb.op("act", lambda e: e.activation(out=sq[:], in_=yc[:], func=AF.Square), reads=[ycb], writes=[sqb])
                pv, pvb = pT.next()
                kb.op("pe", lambda e: e.matmul(pv[:], g["blockones"][:], sq[:], start=True, stop=True), reads=[sqb, cb], writes=[pvb])
                sd, sdb = wk.next()
                kb.op("act", lambda e: e.activation(out=sd[:], in_=pv[:], func=AF.Sqrt, bias=64e-5, scale=1.0 / 64), reads=[pvb], writes=[sdb])
                kb.op("dve", lambda e: e.reciprocal(out=sd[:], in_=sd[:]), reads=[sdb], writes=[sdb])
                kb.op("dve", lambda e: e.tensor_tensor(out=yc[:], in0=yc[:], in1=sd[:], op=ALU.mult), reads=[ycb, sdb], writes=[ycb])
                g1, g1b = gr.next(); g2, g2b = gr.next()
                kb.dma("sp", g1[:], S["G1"][c, :, t0:t0 + 512], reads=[Sb["G1"]], writes=[g1b])
                kb.dma("act", g2[:], S["G2"][c, :, t0:t0 + 512], reads=[Sb["G2"]], writes=[g2b])
                kb.op("dve", lambda e: e.tensor_tensor(out=yc[:], in0=yc[:], in1=g1[:], op=ALU.mult), reads=[ycb, g1b], writes=[ycb])
                kb.op("dve", lambda e: e.tensor_tensor(out=obH[:, c, :], in0=yc[:], in1=g2[:], op=ALU.add), reads=[ycb, g2b], writes=[obHb])
            kb.dma("pool", oaT[:], S["OAT"][:, :, t0:t0 + 512].rearrange("h p t -> p h t"), reads=[Sb["OAT"]], writes=[oaTb])
            for dc in range(8):
                wa, wab = wr.next()
                kb.dma("pool", wa[:], wav[:, :, dc * 128:(dc + 1) * 128], writes=[wab])
                pa, pab = pT.next()
                for kc in range(8):
                    kb.op("pe", lambda e: e.matmul(pa[:], wa[:, kc, :], oaT[:, kc, :], start=(kc == 0), stop=(kc == 7)), reads=[wab, oaTb], writes=[pab])
                wb_, wbb = wr.next()
                kb.dma("pool", wb_[:], wbv[:, :, dc * 128:(dc + 1) * 128], writes=[wbb])
                pb_, pbb = pT.next()
                for kc in range(8):
                    kb.op("pe", lambda e: e.matmul(pb_[:], wb_[:, kc, :], obH[:, kc, :], start=(kc == 0), stop=(kc == 7)), reads=[wbb, obHb], writes=[pbb])
                ga, gab = gr.next(); gb_, gbb = gr.next()
                kb.dma("sp", ga[:], S["GA"][dc, :, t0:t0 + 512], reads=[Sb["GA"]], writes=[gab])
                kb.dma("act", gb_[:], S["GA"][8 + dc, :, t0:t0 + 512], reads=[Sb["GA"]], writes=[gbb])
                m1, m1b = wk.next()
                kb.op("dve", lambda e: e.tensor_tensor(out=m1[:], in0=pa[:], in1=ga[:], op=ALU.mult), reads=[pab, gab], writes=[m1b])
                m2, m2b = wk.next()
                kb.op("dve", lambda e: e.tensor_tensor(out=m2[:], in0=pb_[:], in1=gb_[:], op=ALU.mult), reads=[pbb, gbb], writes=[m2b])
                kb.op("dve", lambda e: e.tensor_tensor(out=mg[:, dc, :], in0=m1[:], in1=m2[:], op=ALU.add), reads=[m1b, m2b], writes=[mgb])
            for hf in range(2):
                wt, wtb = wo.next()
                kb.dma("pool", wt[:], wov[:, :, hf * 512:(hf + 1) * 512], writes=[wtb])
                for ti in range(4):
                    tt0 = t0 + ti * 128
                    for kc in range(8):
                        kb.op("pe", lambda e: e.matmul(pX[:, 0:512], mg[:, kc, ti * 128:(ti + 1) * 128], wt[:, kc, :], start=(kc == 0), stop=(kc == 7)),
                              reads=[mgb, wtb], writes=[pXb])
                    xt, xtb = xr.next()
                    kb.dma("act", xt[:, 0:512], I["xtok"][tt0:tt0 + 128, hf * 512:(hf + 1) * 512], writes=[xtb])
                    x1, x1b = x1r.next()
                    kb.op("dve", lambda e: e.tensor_tensor(out=x1[:, 0:512], in0=pX[:, 0:512], in1=mrow[:, 0, hf * 512:(hf + 1) * 512], op=ALU.mult),
                          reads=[pXb, mrowb], writes=[x1b])
                    kb.op("dve", lambda e: e.tensor_tensor(out=x1[:, 0:512], in0=x1[:, 0:512], in1=xt[:, 0:512], op=ALU.add), reads=[x1b, xtb], writes=[x1b])
                    kb.dma("sp", S["X1"][tt0:tt0 + 128, hf * 512:(hf + 1) * 512], x1[:, 0:512], reads=[x1b], writes=[Sb["X1"]])
        kb.barrier(list(self.Sb.values()))
        es.close()

    def phaseE2(self):
        kb, nc, I, g, S, Sb = self.kb, self.nc, self.I, self.g, self.S, self.Sb
        cb, mrow, mrowb = g["cb"], g["mrow"], g["mrowb"]
        es = ExitStack()
        sbt = lambda n, s, dt=F32: es.enter_context(nc.sbuf_tensor("e2_" + n, list(s), dt))
        NEG = -1.0e30
        keysT = sbt("keysT", [128, 16, 128]); keysTb = Buf()
        kb.dma("sp", keysT[:], I["peer_keysT"].rearrange("j q k -> q j k"), writes=[keysTb])
        x1r = Ring(nc, es, "e2_x1", [128, 1024], F32, 3)
        h2r = Ring(nc, es, "e2_h2", [128, 1024], F32, 2)
        junk = sbt("junk", [128, 1024]); junkb = Buf()
        junkp = sbt("junkp", [128, 1024]); junkpb = Buf()
        h2T = sbt("h2T", [128, 8, 128]); h2Tb = Buf()
        qT = sbt("qT", [128, 16, 128]); qTb = Buf()
        wqr = Ring(nc, es, "e2_wq", [128, 8, 128], F32, 3)
        sc = sbt("sc", [128, 16, 128]); scb = Buf()
        sc2 = sbt("sc2", [128, 16, 128]); sc2b = Buf()
        sv = sbt("sv", [128, 16, 16]); svb = Buf()
        si = sbt("si", [128, 16, 16], U32); sib = Buf()
        sif = sbt("sif", [128, 16, 16]); sifb = Buf()
        cand = sbt("cand", [128, 8, 256]); candb = Buf()
        cand2 = sbt("cand2", [128, 8, 256]); cand2b = Buf()
        cidx = sbt("cidx", [128, 8, 256]); cidxb = Buf()
        tops = sbt("tops", [128, 8, 16]); topsb = Buf()
        pos = sbt("pos", [128, 8, 16], U32); posb = Buf()
        posf = sbt("posf", [128, 8, 16]); posfb = Buf()
        posg = sbt("posg", [128, 8, 16]); posgb = Buf()
        pa_ = sbt("pa_", [128, 8, 16], U32); pab_ = Buf()
        pb_ = sbt("pb_", [128, 8, 16], U32); pbb_ = Buf()
        sel1 = sbt("sel1", [128, 8, 16]); sel1b = Buf()
        sel2 = sbt("sel2", [128, 8, 16]); sel2b = Buf()
        eidx = sbt("eidx", [128, 128]); eidxb = Buf()
        eT = sbt("eT", [128, 128]); eTb = Buf()
        gate = sbt("gate", [128, 8, 16]); gateb = Buf()
        sm = sbt("sm", [128, 32]); smb = Buf()
        jk = sbt("jk", [128, 256]); jkb = Buf()
        gateT = [sbt(f"gateT{i}", [128, 128]) for i in range(2)]; gateTb = [Buf(), Buf()]
        idxT = [sbt(f"idxT{i}", [128, 128], U32) for i in range(2)]; idxTb = [Buf(), Buf()]
        dots = sbt("dots", [128, 128]); dotsb = Buf()
        coef = sbt("coef", [128, 128]); coefb = Buf()
        gw = Ring(nc, es, "e2_gw", [128, 128], F32, 4)
        ugr = Ring(nc, es, "e2_ug", [128, 1024], F32, 7)
        vgr = Ring(nc, es, "e2_vg", [128, 1024], BF16, 7)
        xbr = Ring(nc, es, "e2_xb", [128, 1024], BF16, 6)
        h2br = Ring(nc, es, "e2_h2b", [128, 1024], BF16, 2)
        Wr = Ring(nc, es, "e2_W", [128, 128], BF16, 4)
        outr = Ring(nc, es, "e2_o", [128, 1024], F32, 2)
        pT = Ring(nc, es, "e2_p", [128, 512], F32, 3, psum=True)
        pbig = es.enter_context(nc.psum_tensor("e2_pbig", [128, 1024], F32)); pbigb = Buf()
        pOut = es.enter_context(nc.psum_tensor("e2_pOut", [128, 1024], F32)); pOutb = Buf()
        for i in (range(128) if not getattr(self, "pvb_done", False) else ()):
            tf, tfb = ugr.next()
            kb.dma("sp" if i % 2 else "act", tf[:], I["peer_v"][i * 128:(i + 1) * 128, :], writes=[tfb])
            tb, tbb = vgr.next()
            kb.op("act" if i % 2 else "dve", lambda e: (e.copy(out=tb[:], in_=tf[:]) if i % 2 else e.tensor_copy(out=tb[:], in_=tf[:])),
                  reads=[tfb], writes=[tbb])
            kb.dma("pool", S["PVB"][i * 128:(i + 1) * 128, :], tb[:], reads=[tbb], writes=[Sb["PVB"]])
        wqv = I["peer_wq"].rearrange("(kc p) n -> p kc n", p=128)
        sv4 = sv[:].rearrange("p (h two) k -> p h two k", two=2)
        sif4 = sif[:].rearrange("p (h two) k -> p h two k", two=2)
        c4 = lambda t: t[:].rearrange("p h (a b) -> p h a b", b=16)
        res = {}

        def prep(ti):
            tt0 = ti * 128
            par = ti % 2
            x1, x1b = x1r.next()
            kb.dma("sp", x1[:], S["X1"][tt0:tt0 + 128, :], reads=[Sb["X1"]], writes=[x1b])
            kb.op("act", lambda e: e.activation(out=junkp[:], in_=x1[:], func=AF.Square, accum_out=sm[:, 0:1]), reads=[x1b], writes=[junkpb, smb], fold=False)
            kb.op("act", lambda e: e.activation(out=sm[:, 1:2], in_=sm[:, 0:1], func=AF.Sqrt, bias=EPS, scale=1.0 / D), reads=[smb], writes=[smb])
            kb.op("dve", lambda e: e.reciprocal(out=sm[:, 2:3], in_=sm[:, 1:2]), reads=[smb], writes=[smb])
            yield
            h2, h2b = h2r.next()
            kb.op("dve", lambda e: e.scalar_tensor_tensor(out=h2[:], in0=x1[:], scalar=sm[:, 2:3], in1=mrow[:, 2, :], op0=ALU.mult, op1=ALU.mult),
                  reads=[x1b, smb, mrowb], writes=[h2b])
            yield
            kb.op("dve", lambda e: e.tensor_tensor(out=h2[:], in0=h2[:], in1=mrow[:, 1, :], op=ALU.add), reads=[h2b, mrowb], writes=[h2b])
            yield
            h2bf, h2bfb = h2br.next()
            kb.op("act", lambda e: e.copy(out=h2bf[:], in_=h2[:]), reads=[h2b], writes=[h2bfb])
            kb.dma("act", S["H2"][tt0:tt0 + 128, :], h2bf[:], reads=[h2bfb], writes=[Sb["H2"]])
            for c in range(8):
                kb.op("pe", lambda e: e.transpose(pbig[:, c * 128:(c + 1) * 128], h2[:, c * 128:(c + 1) * 128], g["ident"][:]),
                      reads=[h2b, cb], writes=[pbigb])
            kb.op("act", lambda e: e.copy(out=h2T[:], in_=pbig[:].rearrange("p (c t) -> p c t", t=128)), reads=[pbigb], writes=[h2Tb])
            yield
            for j4 in range(4):
                pq, pqb = pT.next()
                for jj in range(4):
                    j = j4 * 4 + jj
                    wq, wqb = wqr.next()
                    kb.dma("sp" if j % 2 else "act", wq[:], wqv[:, :, j * 128:(j + 1) * 128], writes=[wqb])
                    for kc in range(8):
                        kb.op("pe", lambda e: e.matmul(pq[:, jj * 128:(jj + 1) * 128], wq[:, kc, :], h2T[:, kc, :], start=(kc == 0), stop=(kc == 7)),
                              reads=[wqb, h2Tb], writes=[pqb])
                    yield
                kb.op("act", lambda e: e.copy(out=qT[:, j4 * 4:(j4 + 1) * 4, :], in_=pq[:].rearrange("p (c t) -> p c t", t=128)), reads=[pqb], writes=[qTb])
            for j4 in range(4):
                psc, pscb = pT.next()
                for jj in range(4):
                    j = j4 * 4 + jj
                    kb.op("pe", lambda e: e.matmul(psc[:, jj * 128:(jj + 1) * 128], qT[:, j, :], keysT[:, j, :], start=True, stop=True),
                          reads=[qTb, keysTb], writes=[pscb])
                kb.op("act", lambda e: e.copy(out=sc[:, j4 * 4:(j4 + 1) * 4, :], in_=psc[:].rearrange("p (c t) -> p c t", t=128)), reads=[pscb], writes=[scb])
                yield
            for j in range(16):
                kb.op("dve", lambda e: e.max(out=sv[:, j, 0:8], in_=sc[:, j, :]), reads=[scb], writes=[svb])
                kb.op("dve", lambda e: e.max_index(out=si[:, j, 0:8], in_max=sv[:, j, 0:8], in_values=sc[:, j, :]), reads=[scb, svb], writes=[sib])
                yield
                kb.op("dve", lambda e: e.match_replace(out=sc2[:, j, :], in_to_replace=sv[:, j, 0:8], in_values=sc[:, j, :], imm_value=NEG),
                      reads=[scb, svb], writes=[sc2b])
                kb.op("dve", lambda e: e.max(out=sv[:, j, 8:16], in_=sc2[:, j, :]), reads=[sc2b], writes=[svb])
                yield
                kb.op("dve", lambda e: e.max_index(out=si[:, j, 8:16], in_max=sv[:, j, 8:16], in_values=sc2[:, j, :]), reads=[sc2b, svb], writes=[sib])
                yield
            kb.op("dve", lambda e: e.tensor_copy(out=sif[:], in_=si[:]), reads=[sib], writes=[sifb])
            kb.op("dve", lambda e: e.tensor_tensor(out=c4(cand), in0=sv4[:, :, 0, :].unsqueeze(3).to_broadcast([128, 8, 16, 16]),
                                                   in1=sv4[:, :, 1, :].unsqueeze(2).to_broadcast([128, 8, 16, 16]), op=ALU.add),
                  reads=[svb], writes=[candb])
            yield
            kb.op("dve", lambda e: e.tensor_scalar(out=sif4[:, :, 0, :], in0=sif4[:, :, 0, :], scalar1=128.0, scalar2=None, op0=ALU.mult),
                  reads=[sifb], writes=[sifb])
            kb.op("dve", lambda e: e.tensor_tensor(out=c4(cidx), in0=sif4[:, :, 0, :].unsqueeze(3).to_broadcast([128, 8, 16, 16]),
                                                   in1=sif4[:, :, 1, :].unsqueeze(2).to_broadcast([128, 8, 16, 16]), op=ALU.add),
                  reads=[sifb], writes=[cidxb])
            yield
            for h in range(8):
                kb.op("dve", lambda e: e.max(out=tops[:, h, 0:8], in_=cand[:, h, :]), reads=[candb], writes=[topsb])
                kb.op("dve", lambda e: e.max_index(out=pos[:, h, 0:8], in_max=tops[:, h, 0:8], in_values=cand[:, h, :]), reads=[candb, topsb], writes=[posb])
                yield
                kb.op("dve", lambda e: e.match_replace(out=cand2[:, h, :], in_to_replace=tops[:, h, 0:8], in_values=cand[:, h, :], imm_value=NEG),
                      reads=[candb, topsb], writes=[cand2b])
                kb.op("dve", lambda e: e.max(out=tops[:, h, 8:16], in_=cand2[:, h, :]), reads=[cand2b], writes=[topsb])
                yield
                kb.op("dve", lambda e: e.max_index(out=pos[:, h, 8:16], in_max=tops[:, h, 8:16], in_values=cand2[:, h, :]), reads=[cand2b, topsb], writes=[posb])
                yield
            kb.op("dve", lambda e: e.tensor_scalar(out=pa_[:], in0=pos[:], scalar1=4, scalar2=None, op0=ALU.logical_shift_right), reads=[posb], writes=[pab_])
            kb.op("dve", lambda e: e.tensor_scalar(out=pb_[:], in0=pos[:], scalar1=15, scalar2=None, op0=ALU.bitwise_and), reads=[posb], writes=[pbb_])
            kb.op("dve", lambda e: e.tensor_copy(out=posf[:], in_=pa_[:]), reads=[pab_], writes=[posfb])
            kb.op("dve", lambda e: e.tensor_copy(out=posg[:], in_=pb_[:]), reads=[pbb_], writes=[posgb])
            yield
            i16 = g["iota"][:, 0:16].unsqueeze(1).unsqueeze(1).to_broadcast([128, 8, 16, 16])
            for half, pf, pfb, acc, accb in ((0, posf, posfb, sel1, sel1b), (1, posg, posgb, sel2, sel2b)):
                kb.op("dve", lambda e: e.tensor_tensor(out=c4(cand2), in0=i16, in1=pf[:].unsqueeze(3).to_broadcast([128, 8, 16, 16]), op=ALU.is_equal),
                      reads=[pfb, cb], writes=[cand2b])
                yield
                kb.op("dve", lambda e: e.tensor_tensor(out=c4(cand2), in0=c4(cand2), in1=sif4[:, :, half, :].unsqueeze(2).to_broadcast([128, 8, 16, 16]), op=ALU.mult),
                      reads=[cand2b, sifb], writes=[cand2b])
                yield
                kb.op("dve", lambda e: e.tensor_reduce(out=acc[:], in_=c4(cand2), axis=AX.X, op=ALU.add), reads=[cand2b], writes=[accb])
                yield
            kb.op("dve", lambda e: e.tensor_tensor(out=eidx[:].rearrange("p (h k) -> p h k", k=16), in0=sel1[:], in1=sel2[:], op=ALU.add),
                  reads=[sel1b, sel2b], writes=[eidxb])
            yield
            kb.op("dve", lambda e: e.tensor_scalar(out=sm[:, 8:16], in0=tops[:, :, 0], scalar1=-1.0, scalar2=None, op0=ALU.mult), reads=[topsb], writes=[smb])
            for h in range(8):
                kb.op("act", lambda e: e.activation(out=gate[:, h, :], in_=tops[:, h, :], func=AF.Exp, bias=sm[:, 8 + h:9 + h], accum_out=sm[:, 16 + h:17 + h]),
                      reads=[topsb, smb], writes=[gateb, smb], fold=False)
            yield
            kb.op("dve", lambda e: e.reciprocal(out=sm[:, 24:32], in_=sm[:, 16:24]), reads=[smb], writes=[smb])
            kb.op("dve", lambda e: e.tensor_tensor(out=gate[:], in0=gate[:], in1=sm[:, 24:32].unsqueeze(2).to_broadcast([128, 8, 16]), op=ALU.mult),
                  reads=[gateb, smb], writes=[gateb])
            yield
            pg, pgb = pT.next()
            kb.op("pe", lambda e: e.transpose(pg[:, 0:128], gate[:].rearrange("p h k -> p (h k)"), g["ident"][:]), reads=[gateb, cb], writes=[pgb])
            kb.op("pe", lambda e: e.transpose(pg[:, 128:256], eidx[:], g["ident"][:]), reads=[eidxb, cb], writes=[pgb])
            kb.op("act", lambda e: e.copy(out=gateT[par][:], in_=pg[:, 0:128]), reads=[pgb], writes=[gateTb[par]])
            kb.op("act", lambda e: e.copy(out=eT[:], in_=pg[:, 128:256]), reads=[pgb], writes=[eTb])
            kb.op("dve", lambda e: e.tensor_copy(out=idxT[par][:], in_=eT[:]), reads=[eTb], writes=[idxTb[par]])
            res[ti] = (x1, x1b)
            yield

        def drain(gen):
            if gen is not None:
                for _ in gen:
                    pass

        def step(gen, n):
            if gen is not None:
                for _ in range(n):
                    next(gen, None)

        NTILE = NX // 128
        drain(prep(0))
        for ti in range(NTILE):
            tt0 = ti * 128
            par = ti % 2
            x1, x1b = res.pop(ti)
            idxT_, idxTb_, gateT_, gateTb_ = idxT[par], idxTb[par], gateT[par], gateTb[par]
            gen = prep(ti + 1) if (ti + 1 < NTILE and PEER_PIPELINE) else None
            for tok in range(128):
                ug, ugb = ugr.next()
                kb.dma("pool", None, None, reads=[idxTb_], writes=[ugb],
                       fn=lambda e: e.indirect_dma_start(out=ug[:], out_offset=None, in_=I["peer_u"],
                                                         in_offset=bass.IndirectOffsetOnAxis(ap=idxT_[:, tok:tok + 1], axis=0)))
                xb, xbb = xbr.next()
                kb.dma("sp" if tok % 2 else "act", xb[:], S["H2"][tt0 + tok, :].partition_broadcast(128), reads=[Sb["H2"]], writes=[xbb])
                kb.op("dve", lambda e: e.scalar_tensor_tensor(out=junk[:], in0=ug[:], scalar=1.0, in1=xb[:], op0=ALU.mult, op1=ALU.mult,
                                                              accum_out=dots[:, tok:tok + 1]), reads=[ugb, xbb], writes=[junkb, dotsb], fold=False)
                step(gen, 2)
            t1, t1b = gw.next(); t2, t2b = gw.next()
            kb.op("dve", lambda e: e.tensor_tensor(out=t1[:], in0=dots[:], in1=dots[:], op=ALU.mult), reads=[dotsb], writes=[t1b])
            kb.op("dve", lambda e: e.tensor_scalar(out=t1[:], in0=t1[:], scalar1=0.044715, scalar2=1.0, op0=ALU.mult, op1=ALU.add), reads=[t1b], writes=[t1b])
            kb.op("dve", lambda e: e.tensor_tensor(out=t1[:], in0=t1[:], in1=dots[:], op=ALU.mult), reads=[t1b, dotsb], writes=[t1b])
            kb.op("act", lambda e: e.activation(out=t2[:], in_=t1[:], func=AF.Tanh, scale=0.7978845608028654), reads=[t1b], writes=[t2b])
            kb.op("dve", lambda e: e.tensor_scalar(out=t2[:], in0=t2[:], scalar1=1.0, scalar2=0.5, op0=ALU.add, op1=ALU.mult), reads=[t2b], writes=[t2b])
            kb.op("dve", lambda e: e.tensor_tensor(out=t2[:], in0=t2[:], in1=dots[:], op=ALU.mult), reads=[t2b, dotsb], writes=[t2b])
            kb.op("dve", lambda e: e.tensor_tensor(out=coef[:], in0=t2[:], in1=gateT_[:], op=ALU.mult), reads=[t2b, gateTb_], writes=[coefb])
            for tok in range(128):
                vg, vgb = vgr.next()
                kb.dma("pool", None, None, reads=[idxTb_, Sb["PVB"]], writes=[vgb],
                       fn=lambda e: e.indirect_dma_start(out=vg[:], out_offset=None, in_=S["PVB"],
                                                         in_offset=bass.IndirectOffsetOnAxis(ap=idxT_[:, tok:tok + 1], axis=0)))
                W, Wb = Wr.next()
                kb.op("act", lambda e: e.activation(out=W[:], in_=g["Cw"][:, 127 - tok:255 - tok], func=AF.Identity, scale=coef[:, tok:tok + 1]),
                      reads=[coefb, cb], writes=[Wb])
                for hf in range(2):
                    kb.op("pe", lambda e: e.matmul(pOut[:, hf * 512:(hf + 1) * 512], W[:], vg[:, hf * 512:(hf + 1) * 512],
                                                   start=(tok == 0), stop=(tok == 127)), reads=[Wb, vgb], writes=[pOutb])
                step(gen, 2)
            ot, otb = outr.next()
            kb.op("dve", lambda e: e.tensor_tensor(out=ot[:], in0=pOut[:], in1=mrow[:, 3, :], op=ALU.mult), reads=[pOutb, mrowb], writes=[otb])
            kb.op("dve", lambda e: e.tensor_tensor(out=ot[:], in0=ot[:], in1=x1[:], op=ALU.add), reads=[otb, x1b], writes=[otb])
            kb.dma("sp", self.out[tt0:tt0 + 128, :], ot[:], reads=[otb])
            drain(gen)
        kb.barrier(list(self.Sb.values()))
        es.close()

_W_KEYS = ["w_mod", "b_mod", "norm1_g", "w_in", "q_norm_g", "k_norm_g", "diff_lambda", "diff_out_g", "rw_shift",
           "rw_w0", "rw_w_up", "rw_a0", "rw_a_up", "rw_g_up", "rw_k_k", "rw_k_a", "rw_ln_g", "rw_ln_b",
           "w_branch_a", "w_branch_b", "w_out", "norm2_g", "peer_wq", "peer_u", "peer_v"]


def prep_shared(inp, consts):
    d = {k: np.ascontiguousarray(np.asarray(inp[k], np.float32)[0]) for k in _W_KEYS}
    d["rw_r_k"] = np.ascontiguousarray(np.asarray(inp["rw_r_k"], np.float32)[0].reshape(D))
    d["peer_keysT"] = np.ascontiguousarray(np.asarray(inp["peer_keys"], np.float32)[0].transpose(0, 1, 3, 2).reshape(16, 128, 128))
    d.update(consts)
    return d


def prep_core(inp, b, shared):
    d = dict(shared)
    x = np.asarray(inp["x"], np.float32)[b]
    ctx = np.asarray(inp["ctx"], np.float32)[b]
    d["xT"] = np.ascontiguousarray(np.concatenate([ctx, x], 0).T)
    d["xtok"] = np.ascontiguousarray(x)
    d["cT"] = np.ascontiguousarray(np.stack([np.asarray(inp["c"], np.float32)[b], np.asarray(inp["c_ctx"], np.float32)], 1))
    return d


_PROG = None


def kernel(**inputs):
    global _PROG
    if _PROG is None:
        _PROG = Prog(debug=False, upto="F")
    shared = prep_shared(inputs, _PROG.cnp)
    in_maps = [prep_core(inputs, b, shared) for b in range(8)]
    res = run_bass_kernel_spmd(_PROG.nc, in_maps, core_ids=list(range(8)))
    return np.stack([np.asarray(r["out"], np.float32) for r in res.results], 0)
```
